# Optimizing a Trainium2 kernel written in Bass

```python
import math, functools
import jax, jax.numpy as jnp
from jax import lax
import numpy as np

D_MODEL = 1024
BATCH = 4
SEQ = 8192
DEPTH = 4

CTX_LEN = 256
GRID_W = 64

HEAD_DIM = 64
CHUNK = 64
GDN_HEADS = 4
GDN_WIDTH = GDN_HEADS * HEAD_DIM
SHORT_CONV = 3
S5_WIDTH = 256
S5_GROUP = 16
S5_GROUPS = S5_WIDTH // S5_GROUP
S5_STATE = 64
HGRN_HEADS = 4
HGRN_WIDTH = HGRN_HEADS * HEAD_DIM
RET_HEADS = 4
RET_WIDTH = RET_HEADS * HEAD_DIM
ROPE_BASE = 10000.0
D_MIX = GDN_WIDTH + S5_WIDTH + HGRN_WIDTH + RET_WIDTH

PROJ_SIZES = (
    3 * GDN_WIDTH,
    GDN_WIDTH,
    2 * GDN_HEADS,
    2 * GDN_HEADS,
    S5_WIDTH,
    HGRN_WIDTH,
    2 * HGRN_WIDTH,
    HGRN_WIDTH,
    HGRN_WIDTH,
    3 * RET_WIDTH,
    RET_WIDTH,
)
D_PROJ = sum(PROJ_SIZES)

FF_DENSE = 2816
N_EXPERTS = 8
TOP_K = 2
FF_EXPERT = 3584
MOE_BLOCK = 128
N_DENSE_LAYERS = (DEPTH + 1) // 2
N_MOE_LAYERS = DEPTH // 2

DEEPNORM_ALPHA = (2.0 * DEPTH) ** 0.25
DEEPNORM_BETA = (8.0 * DEPTH) ** -0.25
LN_EPS = 1e-5
RMS_EPS = 1e-6

kernel_name = "hybrid_parallel_mixer_diffusion_trunk"


def _f32(t):
    return t.astype(jnp.float32)


def _layer_norm(x, g, b):
    xf = _f32(x)
    mu = jnp.mean(xf, -1, keepdims=True)
    var = jnp.mean(jnp.square(xf - mu), -1, keepdims=True)
    return ((xf - mu) * lax.rsqrt(var + LN_EPS) * _f32(g) + _f32(b)).astype(x.dtype)


def _rms_norm(x, w=None):
    y = x * lax.rsqrt(jnp.mean(jnp.square(x), -1, keepdims=True) + RMS_EPS)
    return y if w is None else y * w


def _l2norm(t):
    return t * lax.rsqrt(jnp.sum(t * t, -1, keepdims=True) + RMS_EPS)


def _heads(t, n_heads):
    b, l, _ = t.shape
    return t.reshape(b, l, n_heads, -1).transpose(0, 2, 1, 3)


def _merge(t):
    b, h, l, d = t.shape
    return t.transpose(0, 2, 1, 3).reshape(b, l, h * d)


def _chunk(t):
    return t.reshape(t.shape[:2] + (t.shape[2] // CHUNK, CHUNK) + t.shape[3:])


def _scan_chunks(step, s0, xs):
    s, o = lax.scan(step, s0, tuple(jnp.moveaxis(t, 2, 0) for t in xs))
    o = jnp.moveaxis(o, 0, 2)
    return o.reshape(o.shape[:2] + (-1,) + o.shape[4:]), s


def _short_conv(x, w):
    pad = SHORT_CONV // 2
    return lax.conv_general_dilated(x, w[:, None, :], window_strides=(1,), padding=((pad, pad),),
                                    dimension_numbers=('NWC', 'WIO', 'NWC'),
                                    feature_group_count=x.shape[-1])


def _axial_rotary(t, rows, cols):
    half = t.shape[-1] // 2
    quarter = half // 2
    inv_freq = ROPE_BASE ** (-jnp.arange(quarter, dtype=jnp.float32) / quarter)
    ang = jnp.concatenate([rows[:, None] * inv_freq, cols[:, None] * inv_freq], axis=-1)
    cos, sin = jnp.cos(ang), jnp.sin(ang)
    t1, t2 = t[..., :half], t[..., half:]
    return jnp.concatenate([t1 * cos - t2 * sin, t1 * sin + t2 * cos], axis=-1)


def _bidirectional(run_f, run_b, ctx_f, ctx_b, lat_f, lat_b, axis, need_ctx):
    flip = lambda xs: tuple(jnp.flip(t, axis) for t in xs)
    o_cf, s_cf = run_f(*ctx_f, None)
    o_cb, s_cb = run_b(*flip(ctx_b), None)
    o_lf, _ = run_f(*lat_f, s_cf)
    o_lb, _ = run_b(*flip(lat_b), s_cb)
    o_lat = o_lf + jnp.flip(o_lb, axis)
    o_ctx = (o_cf + jnp.flip(o_cb, axis)) if need_ctx else None
    return o_ctx, o_lat


def _gdn_direction(q, k, v, g, beta, s0):
    bsz, nh, _, dk = q.shape
    dv = v.shape[-1]
    q, k, v, g, beta = (_chunk(t) for t in (q, k, v, g, beta))
    gc = jnp.cumsum(g, axis=-1)
    tri = jnp.tril(jnp.ones((CHUNK, CHUNK), bool))
    decay = jnp.exp(jnp.where(tri, gc[..., :, None] - gc[..., None, :], -jnp.inf))
    kb = k * beta[..., None]
    a = jnp.einsum('bhnik,bhnjk->bhnij', kb, k) * decay
    eye = jnp.broadcast_to(jnp.eye(CHUNK, dtype=a.dtype), a.shape)
    t_inv = lax.linalg.triangular_solve(a, eye, left_side=True, lower=True, unit_diagonal=True)
    u = jnp.einsum('bhnij,bhnjv->bhniv', t_inv, v * beta[..., None])
    w = jnp.einsum('bhnij,bhnjk->bhnik', t_inv, kb * jnp.exp(gc)[..., None])
    qk = jnp.einsum('bhnik,bhnjk->bhnij', q, k) * decay
    if s0 is None:
        s0 = jnp.zeros((bsz, nh, dk, dv), q.dtype)

    def step(s, inp):
        q_c, k_c, u_c, w_c, qk_c, g_c = inp
        v_new = u_c - jnp.einsum('bhck,bhkv->bhcv', w_c, s)
        o = (jnp.einsum('bhck,bhkv->bhcv', q_c * jnp.exp(g_c)[..., None], s)
             + jnp.einsum('bhij,bhjv->bhiv', qk_c, v_new))
        g_last = g_c[..., -1:]
        s = (s * jnp.exp(g_last)[..., None]
             + jnp.einsum('bhck,bhcv->bhkv', k_c * jnp.exp(g_last - g_c)[..., None], v_new))
        return s, o

    return _scan_chunks(step, s0, (q, k, u, w, qk, gc))


def _gdn_mixer(ctx_parts, lat_parts, conv_w, a_log, dt_bias, norm_w, need_ctx):
    def per_dir(t):
        return t.reshape(t.shape[0], t.shape[1], 2, GDN_HEADS).transpose(2, 0, 3, 1)

    def prep(qkv, a, b):
        qkv = jax.nn.silu(_short_conv(qkv, conv_w))
        q, k, v = (_heads(t, GDN_HEADS) for t in jnp.split(qkv, 3, axis=-1))
        q = _l2norm(q) * HEAD_DIM ** -0.5
        k = _l2norm(k)
        g = -jnp.exp(a_log)[:, None, :, None] * jax.nn.softplus(per_dir(a) + dt_bias[:, None, :, None])
        beta = jax.nn.sigmoid(per_dir(b))
        return (q, k, v, g[0], beta[0]), (q, k, v, g[1], beta[1])

    c_qkv, c_z, c_a, c_b = ctx_parts
    l_qkv, l_z, l_a, l_b = lat_parts
    cf, cb = prep(c_qkv, c_a, c_b)
    lf, lb = prep(l_qkv, l_a, l_b)
    o_ctx, o_lat = _bidirectional(_gdn_direction, _gdn_direction, cf, cb, lf, lb, 2, need_ctx)

    def readout(o, z):
        return _merge(_rms_norm(o, norm_w) * jax.nn.silu(_heads(z, GDN_HEADS)))

    return (readout(o_ctx, c_z) if need_ctx else None), readout(o_lat, l_z)


def _s5_direction(lam_bar, b_bar, c_mat, u, h0):
    bu = jnp.einsum('blgh,gph->blgp', u.astype(jnp.complex64), b_bar)
    if h0 is not None:
        bu = bu.at[:, 0].add(lam_bar * h0)
    a = jnp.broadcast_to(lam_bar, bu.shape)

    def combine(e1, e2):
        return e1[0] * e2[0], e2[0] * e1[1] + e2[1]

    _, states = lax.associative_scan(combine, (a, bu), axis=1)
    y = jnp.real(jnp.einsum('blgp,ghp->blgh', states, c_mat))
    return y, states[:, -1]


def _s5_mixer(u_ctx, u_lat, a_re, a_im, log_step, b_re, b_im, c_re, c_im, d_skip, glu_w, glu_b,
              need_ctx):
    lam = lax.complex(a_re, a_im)
    lam_bar = jnp.exp(lam * jnp.exp(log_step)[..., None])
    b_bar = ((lam_bar - 1.0) / lam)[..., None] * lax.complex(b_re, b_im)
    c_mat = lax.complex(c_re, c_im)
    run_f = functools.partial(_s5_direction, lam_bar[0], b_bar[0], c_mat[0])
    run_b = functools.partial(_s5_direction, lam_bar[1], b_bar[1], c_mat[1])
    grp = lambda t: t.reshape(t.shape[:2] + (S5_GROUPS, S5_GROUP))
    uc, ul = grp(u_ctx), grp(u_lat)
    y_ctx, y_lat = _bidirectional(run_f, run_b, (uc,), (uc,), (ul,), (ul,), 1, need_ctx)

    def readout(y, u):
        y = jax.nn.gelu(y.reshape(u.shape) + d_skip * u)
        return y * jax.nn.sigmoid(y @ glu_w + glu_b)

    return (readout(y_ctx, u_ctx) if need_ctx else None), readout(y_lat, u_lat)


def _hgrn2_direction(q, k, v, logf, s0):
    bsz, nh, _, dk = q.shape
    dv = v.shape[-1]
    q, k, v, logf = (_chunk(t) for t in (q, k, v, logf))
    gc = jnp.cumsum(logf, axis=3)
    tri = jnp.tril(jnp.ones((CHUNK, CHUNK), bool))[:, :, None]
    if s0 is None:
        s0 = jnp.zeros((bsz, nh, dk, dv), q.dtype)

    def step(s, inp):
        q_c, k_c, v_c, g_c = inp
        rel = jnp.exp(jnp.where(tri, g_c[:, :, :, None, :] - g_c[:, :, None, :, :], -jnp.inf))
        att = jnp.einsum('bhik,bhjk,bhijk->bhij', q_c, k_c, rel)
        o = (jnp.einsum('bhck,bhkv->bhcv', q_c * jnp.exp(g_c), s)
             + jnp.einsum('bhij,bhjv->bhiv', att, v_c))
        g_last = g_c[:, :, -1:, :]
        s = (s * jnp.exp(g_last[:, :, 0, :])[..., None]
             + jnp.einsum('bhck,bhcv->bhkv', k_c * jnp.exp(g_last - g_c), v_c))
        return s, o

    return _scan_chunks(step, s0, (q, k, v, gc))


def _hgrn2_mixer(ctx_parts, lat_parts, lower_bound, norm_w, need_ctx):
    lb = lower_bound.reshape(HGRN_HEADS, 1, -1)

    def prep(q, f, i):
        q = jax.nn.silu(_heads(q, HGRN_HEADS))
        i = _heads(i, HGRN_HEADS)
        dirs = []
        for fz in jnp.split(f, 2, axis=-1):
            logf = jnp.logaddexp(jnp.log(lb), jnp.log1p(-lb) + jax.nn.log_sigmoid(_heads(fz, HGRN_HEADS)))
            dirs.append((q, -jnp.expm1(logf), i, logf))
        return dirs

    c_q, c_f, c_i, c_g = ctx_parts
    l_q, l_f, l_i, l_g = lat_parts
    cf, cb = prep(c_q, c_f, c_i)
    lf, lb_ = prep(l_q, l_f, l_i)
    o_ctx, o_lat = _bidirectional(_hgrn2_direction, _hgrn2_direction, cf, cb, lf, lb_, 2, need_ctx)

    def readout(o, g):
        return _merge(_rms_norm(o, norm_w) * jax.nn.silu(_heads(g, HGRN_HEADS)))

    return (readout(o_ctx, c_g) if need_ctx else None), readout(o_lat, l_g)


def _retention_direction(log_gamma, q, k, v, s0):
    bsz, nh, _, dk = q.shape
    dv = v.shape[-1]
    q, k, v = (_chunk(t) for t in (q, k, v))
    idx = jnp.arange(CHUNK, dtype=jnp.float32)
    tri = jnp.tril(jnp.ones((CHUNK, CHUNK), bool))
    decay = jnp.exp(jnp.where(tri, (idx[:, None] - idx[None, :]) * log_gamma[:, None, None], -jnp.inf))
    intra = jnp.einsum('bhnij,bhnjv->bhniv',
                       jnp.einsum('bhnik,bhnjk->bhnij', q, k) * decay[None, :, None], v)
    q_dec = jnp.exp((idx + 1.0) * log_gamma[:, None])
    k_dec = jnp.exp((CHUNK - 1.0 - idx) * log_gamma[:, None])
    c_dec = jnp.exp(CHUNK * log_gamma)
    if s0 is None:
        s0 = jnp.zeros((bsz, nh, dk, dv), q.dtype)

    def step(s, inp):
        q_c, k_c, v_c = inp
        o = jnp.einsum('bhck,bhkv->bhcv', q_c * q_dec[None, :, :, None], s)
        s = s * c_dec[None, :, None, None] + jnp.einsum('bhck,bhcv->bhkv', k_c * k_dec[None, :, :, None], v_c)
        return s, o

    inter, s = _scan_chunks(step, s0, (q, k, v))
    return intra.reshape(inter.shape) + inter, s


def _retention_mixer(ctx_parts, lat_parts, rows, cols, decay_param, need_ctx):
    log_gamma = -jnp.exp(decay_param)

    def prep(qkv, rotate):
        q, k, v = (_heads(t, RET_HEADS) for t in jnp.split(qkv, 3, axis=-1))
        if rotate:
            q, k = _axial_rotary(q, rows, cols), _axial_rotary(k, rows, cols)
        return (q, k * HEAD_DIM ** -0.5, v)

    c_qkv, c_g = ctx_parts
    l_qkv, l_g = lat_parts
    cin, lin = prep(c_qkv, False), prep(l_qkv, True)
    run_f = functools.partial(_retention_direction, log_gamma[0])
    run_b = functools.partial(_retention_direction, log_gamma[1])
    o_ctx, o_lat = _bidirectional(run_f, run_b, cin, cin, lin, lin, 2, need_ctx)

    def readout(o, g):
        return _merge(_rms_norm(o) * jax.nn.silu(_heads(g, RET_HEADS)))

    return (readout(o_ctx, c_g) if need_ctx else None), readout(o_lat, l_g)


def _swiglu(h, w1, w3, w2):
    return (jax.nn.silu(h @ w1) * (h @ w3)) @ w2


def _moe_swiglu(h, router, w1, w3, w2):
    shape = h.shape
    tok = h.reshape(-1, shape[-1])
    n_tok = tok.shape[0]
    n_asg = n_tok * TOP_K
    logits = _f32(tok @ router)
    top_val, top_idx = lax.top_k(logits, TOP_K)
    gates = jax.nn.softmax(top_val, axis=-1)
    flat_e = top_idx.reshape(-1)
    order = jnp.argsort(flat_e)
    sorted_e = flat_e[order]
    counts = jnp.bincount(flat_e, length=N_EXPERTS)
    padded = (counts + MOE_BLOCK - 1) // MOE_BLOCK * MOE_BLOCK
    start = jnp.cumsum(counts) - counts
    pad_end = jnp.cumsum(padded)
    pad_start = pad_end - padded
    dest = pad_start[sorted_e] + jnp.arange(n_asg) - start[sorted_e]
    n_blocks = -(-n_asg // MOE_BLOCK) + N_EXPERTS
    n_rows = n_blocks * MOE_BLOCK
    row_tok = jnp.full((n_rows,), n_tok, jnp.int32).at[dest].set((order // TOP_K).astype(jnp.int32))
    row_gate = jnp.zeros((n_rows,), jnp.float32).at[dest].set(gates.reshape(-1)[order])
    block_expert = jnp.minimum(
        jnp.sum(jnp.arange(n_blocks)[:, None] * MOE_BLOCK >= pad_end[None, :], axis=1), N_EXPERTS - 1)
    tok_pad = jnp.concatenate([tok, jnp.zeros((1, tok.shape[1]), tok.dtype)], axis=0)
    xb = tok_pad[row_tok].reshape(n_blocks, MOE_BLOCK, -1)

    def expert_block(args):
        xblk, e = args
        return (jax.nn.silu(xblk @ w1[e]) * (xblk @ w3[e])) @ w2[e]

    yb = lax.map(expert_block, (xb, block_expert)).reshape(n_rows, -1)
    out = jnp.zeros_like(tok_pad).at[row_tok].add(yb * row_gate[:, None].astype(yb.dtype))
    return out[:n_tok].reshape(shape)


def _split_proj(p):
    return jnp.split(p, np.cumsum(PROJ_SIZES)[:-1].tolist(), axis=-1)


def setup_inputs(seed: int = 0) -> dict:
    key = jax.random.key(seed)
    ks = iter(jax.random.split(key, 48))
    nrm = lambda shape, scale: jax.random.normal(next(ks), shape, jnp.float32) * scale
    unif = lambda shape, lo, hi: jax.random.uniform(next(ks), shape, jnp.float32, lo, hi)
    D, L = D_MODEL, DEPTH
    dt_init = jnp.exp(unif((L, 2, GDN_HEADS), math.log(1e-3), math.log(1e-1)))
    ret_init = jnp.log(-jnp.log1p(-(2.0 ** (-5.0 - jnp.arange(RET_HEADS, dtype=jnp.float32)))))
    return {
        "x": nrm((BATCH, SEQ, D), 1.0),
        "c": nrm((BATCH, D), 1.0),
        "ctx": nrm((BATCH, CTX_LEN, D), 1.0),
        "c_ctx": nrm((D,), 1.0),
        "ada_w": nrm((L, D, 6 * D), 0.5 * D ** -0.5),
        "ada_b": nrm((L, 6 * D), 0.01),
        "w_in": nrm((L, D, D_PROJ), D ** -0.5),
        "w_out": nrm((L, D_MIX, D), DEEPNORM_BETA * D_MIX ** -0.5),
        "ln_g": 1.0 + nrm((L, 2, D), 0.01),
        "ln_b": nrm((L, 2, D), 0.01),
        "gdn_conv_w": nrm((L, SHORT_CONV, 3 * GDN_WIDTH), SHORT_CONV ** -0.5),
        "gdn_a_log": jnp.log(unif((L, 2, GDN_HEADS), 1.0, 16.0)),
        "gdn_dt_bias": dt_init + jnp.log(-jnp.expm1(-dt_init)),
        "gdn_norm_w": 1.0 + nrm((L, HEAD_DIM), 0.01),
        "s5_a_re": -0.5 + nrm((L, 2, S5_GROUPS, S5_STATE), 0.01),
        "s5_a_im": jnp.pi * jnp.arange(S5_STATE, dtype=jnp.float32) + nrm((L, 2, S5_GROUPS, S5_STATE), 0.01),
        "s5_log_step": jnp.log(unif((L, 2, S5_GROUPS), 1e-3, 1e-1)),
        "s5_b_re": nrm((L, 2, S5_GROUPS, S5_STATE, S5_GROUP), (2.0 * S5_GROUP) ** -0.5),
        "s5_b_im": nrm((L, 2, S5_GROUPS, S5_STATE, S5_GROUP), (2.0 * S5_GROUP) ** -0.5),
        "s5_c_re": nrm((L, 2, S5_GROUPS, S5_GROUP, S5_STATE), (2.0 * S5_STATE) ** -0.5),
        "s5_c_im": nrm((L, 2, S5_GROUPS, S5_GROUP, S5_STATE), (2.0 * S5_STATE) ** -0.5),
        "s5_d": nrm((L, S5_WIDTH), 1.0),
        "s5_glu_w": nrm((L, S5_WIDTH, S5_WIDTH), S5_WIDTH ** -0.5),
        "s5_glu_b": nrm((L, S5_WIDTH), 0.01),
        "hgrn_lower_bounds": nrm((L, HGRN_WIDTH), 0.1),
        "hgrn_norm_w": 1.0 + nrm((L, HEAD_DIM), 0.01),
        "ret_decay": ret_init + nrm((L, 2, RET_HEADS), 0.05),
        "ffn_w1": nrm((N_DENSE_LAYERS, D, FF_DENSE), D ** -0.5),
        "ffn_w3": nrm((N_DENSE_LAYERS, D, FF_DENSE), D ** -0.5),
        "ffn_w2": nrm((N_DENSE_LAYERS, FF_DENSE, D), DEEPNORM_BETA * FF_DENSE ** -0.5),
        "moe_router": nrm((N_MOE_LAYERS, D, N_EXPERTS), D ** -0.5),
        "moe_w1": nrm((N_MOE_LAYERS, N_EXPERTS, D, FF_EXPERT), D ** -0.5),
        "moe_w3": nrm((N_MOE_LAYERS, N_EXPERTS, D, FF_EXPERT), D ** -0.5),
        "moe_w2": nrm((N_MOE_LAYERS, N_EXPERTS, FF_EXPERT, D), DEEPNORM_BETA * FF_EXPERT ** -0.5),
    }


def reference(x, c, ctx, c_ctx, ada_w, ada_b, w_in, w_out, ln_g, ln_b,
              gdn_conv_w, gdn_a_log, gdn_dt_bias, gdn_norm_w,
              s5_a_re, s5_a_im, s5_log_step, s5_b_re, s5_b_im, s5_c_re, s5_c_im,
              s5_d, s5_glu_w, s5_glu_b,
              hgrn_lower_bounds, hgrn_norm_w, ret_decay,
              ffn_w1, ffn_w3, ffn_w2, moe_router, moe_w1, moe_w3, moe_w2):
    dt = x.dtype
    n_lat = x.shape[1]
    ROWS = n_lat // GRID_W
    rows = jnp.repeat(jnp.arange(ROWS, dtype=jnp.float32), GRID_W)
    cols = jnp.tile(jnp.arange(GRID_W, dtype=jnp.float32), ROWS)
    lb_all = jnp.cumsum(jax.nn.softmax(_f32(hgrn_lower_bounds), axis=0), axis=0)
    lb_all = lb_all - lb_all[0]

    for layer in range(DEPTH):
        need_ctx = layer < DEPTH - 1
        mod_lat = jax.nn.silu(c) @ ada_w[layer] + ada_b[layer]
        mod_ctx = jax.nn.silu(c_ctx) @ ada_w[layer] + ada_b[layer]
        sh1, sc1, gt1, sh2, sc2, gt2 = jnp.split(mod_lat[:, None, :], 6, axis=-1)
        csh1, csc1, cgt1, csh2, csc2, cgt2 = jnp.split(mod_ctx, 6, axis=-1)

        p_lat = _split_proj(_f32((x * (1.0 + sc1) + sh1) @ w_in[layer]))
        p_ctx = _split_proj(_f32((ctx * (1.0 + csc1) + csh1) @ w_in[layer]))
        gdn_c, gdn_l = _gdn_mixer(p_ctx[0:4], p_lat[0:4], _f32(gdn_conv_w[layer]), _f32(gdn_a_log[layer]),
                                  _f32(gdn_dt_bias[layer]), _f32(gdn_norm_w[layer]), need_ctx)
        s5_c, s5_l = _s5_mixer(p_ctx[4], p_lat[4], _f32(s5_a_re[layer]), _f32(s5_a_im[layer]),
                               _f32(s5_log_step[layer]), _f32(s5_b_re[layer]), _f32(s5_b_im[layer]),
                               _f32(s5_c_re[layer]), _f32(s5_c_im[layer]), _f32(s5_d[layer]),
                               _f32(s5_glu_w[layer]), _f32(s5_glu_b[layer]), need_ctx)
        hg_c, hg_l = _hgrn2_mixer(p_ctx[5:9], p_lat[5:9], lb_all[layer], _f32(hgrn_norm_w[layer]), need_ctx)
        rt_c, rt_l = _retention_mixer(p_ctx[9:11], p_lat[9:11], rows, cols, _f32(ret_decay[layer]), need_ctx)

        mix_lat = jnp.concatenate([gdn_l, s5_l, hg_l, rt_l], axis=-1).astype(dt)
        x = _layer_norm(DEEPNORM_ALPHA * x + gt1 * (mix_lat @ w_out[layer]), ln_g[layer, 0], ln_b[layer, 0])
        if need_ctx:
            mix_ctx = jnp.concatenate([gdn_c, s5_c, hg_c, rt_c], axis=-1).astype(dt)
            ctx = _layer_norm(DEEPNORM_ALPHA * ctx + cgt1 * (mix_ctx @ w_out[layer]),
                              ln_g[layer, 0], ln_b[layer, 0])

        j = layer // 2
        if layer % 2 == 0:
            ffn = functools.partial(_swiglu, w1=ffn_w1[j], w3=ffn_w3[j], w2=ffn_w2[j])
        else:
            ffn = functools.partial(_moe_swiglu, router=moe_router[j], w1=moe_w1[j], w3=moe_w3[j], w2=moe_w2[j])
        x = _layer_norm(DEEPNORM_ALPHA * x + gt2 * ffn(x * (1.0 + sc2) + sh2), ln_g[layer, 1], ln_b[layer, 1])
        if need_ctx:
            ctx = _layer_norm(DEEPNORM_ALPHA * ctx + cgt2 * ffn(ctx * (1.0 + csc2) + csh2),
                              ln_g[layer, 1], ln_b[layer, 1])
    return x
```

```python
import math
import numpy as np
from contextlib import ExitStack
import concourse.bass as bass
import concourse.mybir as mybir
from concourse.bass_utils import run_bass_kernel_spmd

F32 = mybir.dt.float32
BF16 = mybir.dt.bfloat16
ALU = mybir.AluOpType
AF = mybir.ActivationFunctionType
AX = mybir.AxisListType

D = 1024
KT = 8
CTX = 256
DPROJ = 3600
FF_DENSE = 2816
FF_EXPERT = 3584
NEXP = 8
ALPHA = 8.0 ** 0.25
GDN_STAGE = 7
LN_EPS = 1e-5
RMS_EPS = 1e-6
TM_GZ, TM_HQ, TM_HF, TM_HI, TM_HG, TM_RQ, TM_RK, TM_RV, TM_RG, TM_AB = 0, 256, 512, 1024, 1280, 1536, 1792, 2048, 2304, 2560
TMW = 2576
TMGROUPS = [(0, 512), (512, 512), (1024, 512), (1536, 512), (2048, 512), (2560, 16)]


class Res:
    __slots__ = ("name", "w", "r", "excl")

    def __init__(self, name, excl=False):
        self.name = name
        self.w = None
        self.r = {}
        self.excl = excl


class T:
    def __init__(self, t, res):
        self.t = t
        self.res = res

    def __getitem__(self, k):
        return self.t[k]


class Rot:
    def __init__(self, items):
        self.items = list(items)
        self.i = 0

    def next(self):
        x = self.items[self.i % len(self.items)]
        self.i += 1
        return x


class Sched:
    NDMA = 6

    def __init__(self, nc, es):
        self.nc = nc
        self.es = es
        self.eng = {"pe": nc.tensor, "dve": nc.vector, "act": nc.scalar, "pool": nc.gpsimd, "sp": nc.sync}
        self.sem, self.cnt, self.seen, self.semobj = {}, {}, {}, {}
        for e in self.eng:
            s = es.enter_context(nc.semaphore("s_" + e))
            self.sem[e] = s
            self.semobj[id(s)] = s
            self.cnt[e] = 0
            self.seen[e] = {}
        self.dsem, self.dcnt = {}, {}
        for q in ("sp", "pool", "act"):
            lst = []
            for i in range(self.NDMA):
                s = es.enter_context(nc.semaphore("d_%s%d" % (q, i)))
                self.semobj[id(s)] = s
                lst.append(s)
            self.dsem[q] = lst
            self.dcnt[q] = 0
        self.ninst = 0
        self.nwait = 0
        self.uid = 0
        for s in self.semobj.values():
            nc.gpsimd.sem_clear(s)
        nc.all_engine_barrier()

    def name(self, base):
        self.uid += 1
        return "%s_%d" % (base, self.uid)

    def sb(self, name, shape, dt=F32, es=None):
        t = (es or self.es).enter_context(self.nc.sbuf_tensor(self.name(name), list(shape), dt))
        return T(t, Res(name))

    def ps(self, name, shape, dt=F32, es=None):
        t = (es or self.es).enter_context(self.nc.psum_tensor(self.name(name), list(shape), dt))
        return T(t, Res(name, excl=True))

    def dram(self, name, shape, dt=F32):
        t = self.nc.dram_tensor(self.name(name), list(shape), dt)
        return T(t.ap(), Res(name))

    def _wait(self, e, ev):
        sid, val = ev
        if self.seen[e].get(sid, 0) >= val:
            return
        self.eng[e].wait_ge(self.semobj[sid], val)
        self.seen[e][sid] = val
        self.nwait += 1

    def _deps(self, e, reads, writes, pe_acc=False):
        evs = []
        for r in reads:
            if r.res.w is not None:
                evs.append(r.res.w)
            if r.res.excl:
                mine = id(self.sem[e]) if e in self.sem else None
                for sid, val in r.res.r.items():
                    if sid != mine:
                        evs.append((sid, val))
        for w in writes:
            if w.res.w is not None:
                if not (pe_acc and e == "pe" and w.res.w[0] == id(self.sem["pe"])):
                    evs.append(w.res.w)
            for sid, val in w.res.r.items():
                evs.append((sid, val))
        need = {}
        for sid, val in evs:
            if self.seen[e].get(sid, 0) >= val:
                continue
            if need.get(sid, 0) < val:
                need[sid] = val
        items = list(need.items())
        for ev in items[:-1]:
            self._wait(e, ev)
        return items[-1] if items else None

    def _attach(self, e, ins, last):
        if last is not None:
            sid, val = last
            ins._wait_ge(self.semobj[sid], val)
            self.seen[e][sid] = val

    def _mark(self, ev, reads, writes):
        sid, val = ev
        for r in reads:
            if r.res.r.get(sid, 0) < val:
                r.res.r[sid] = val
        for w in writes:
            w.res.w = ev
            w.res.r = {}

    def op(self, e, fn, reads=(), writes=(), pe_acc=False):
        last = self._deps(e, reads, writes, pe_acc)
        ins = fn(self.eng[e])
        self._attach(e, ins, last)
        self.cnt[e] += 1
        ins.then_inc(self.sem[e], 1)
        ev = (id(self.sem[e]), self.cnt[e])
        self._mark(ev, reads, writes)
        self.ninst += 1
        return ev

    def dma(self, q, out, in_, reads=(), writes=(), **kw):
        n = self.dcnt[q]
        k = n % self.NDMA
        s = self.dsem[q][k]
        if n >= self.NDMA:
            self._wait(q, (id(s), 16 * (n // self.NDMA)))
        last = self._deps(q, reads, writes)
        ins = self.eng[q].dma_start(out=out, in_=in_, **kw)
        self._attach(q, ins, last)
        ins.then_inc(s, 16)
        self.dcnt[q] += 1
        ev = (id(s), 16 * (n // self.NDMA + 1))
        self._mark(ev, reads, writes)
        self.ninst += 1
        return ev

    def barrier(self):
        evs = []
        for e in self.eng:
            if self.cnt[e] > 0:
                evs.append((id(self.sem[e]), self.cnt[e]))
        for q in self.dsem:
            n = self.dcnt[q]
            for k in range(self.NDMA):
                cntk = (n - k + self.NDMA - 1) // self.NDMA if n > k else 0
                if cntk > 0:
                    evs.append((id(self.dsem[q][k]), 16 * cntk))
        for e in self.eng:
            for ev in evs:
                self._wait(e, ev)

    def drain_all(self):
        for e in self.eng:
            if self.cnt[e] > 0:
                self._wait("sp", (id(self.sem[e]), self.cnt[e]))
        for q in self.dsem:
            n = self.dcnt[q]
            for k in range(self.NDMA):
                cntk = (n - k + self.NDMA - 1) // self.NDMA if n > k else 0
                if cntk > 0:
                    self._wait("sp", (id(self.dsem[q][k]), 16 * cntk))

    def mm(self, out, lhsT, rhs, start, stop, reads, writes):
        return self.op("pe", lambda e: e.matmul(out, lhsT=lhsT, rhs=rhs, start=start, stop=stop),
                       reads=reads, writes=writes, pe_acc=True)

    def tr(self, out, in_, ident, reads, writes):
        return self.op("pe", lambda e: e.transpose(out, in_, ident), reads=reads, writes=writes, pe_acc=True)

    def act(self, out, in_, func, reads, writes, bias=None, scale=None, eng="act"):
        kw = {}
        if bias is not None:
            kw["bias"] = bias
        if scale is not None:
            kw["scale"] = scale
        return self.op("act", lambda e: e.activation(out=out, in_=in_, func=func, **kw), reads=reads, writes=writes)

    def tt(self, e, out, in0, in1, op, reads, writes):
        return self.op(e, lambda g: g.tensor_tensor(out=out, in0=in0, in1=in1, op=op), reads=reads, writes=writes)

    def ts(self, e, out, in0, s1, op0, reads, writes, s2=None, op1=None):
        if op1 is None:
            return self.op(e, lambda g: g.tensor_scalar(out=out, in0=in0, scalar1=s1, scalar2=None, op0=op0),
                           reads=reads, writes=writes)
        return self.op(e, lambda g: g.tensor_scalar(out=out, in0=in0, scalar1=s1, scalar2=s2, op0=op0, op1=op1),
                       reads=reads, writes=writes)

    def stt(self, e, out, in0, scalar, in1, op0, op1, reads, writes):
        return self.op(e, lambda g: g.scalar_tensor_tensor(out=out, in0=in0, scalar=scalar, in1=in1, op0=op0, op1=op1),
                       reads=reads, writes=writes)

    def copy(self, e, out, in_, reads, writes):
        if e == "act":
            return self.op("act", lambda g: g.copy(out=out, in_=in_), reads=reads, writes=writes)
        return self.op(e, lambda g: g.tensor_copy(out=out, in_=in_), reads=reads, writes=writes)

    def memset(self, e, out, val, writes):
        return self.op(e, lambda g: g.memset(out, val), writes=writes)


def host_consts(nlat):
    c = {}
    c["ident"] = np.eye(128, dtype=np.float32)
    c["onesdiv"] = np.full((128, 128), 1.0 / D, np.float32)
    k = np.arange(128)[:, None]
    i = np.arange(128)[None, :]
    same = (k // 64) == (i // 64)
    tri = [same & (k <= i), same & (k >= i)]
    mid = [same & ((k % 64) <= 31), same & ((k % 64) >= 32)]
    allm = same
    loc = k % 64
    loci = i % 64
    submid_f = np.where(loci < 32, 15, 47)
    submid_b = np.where(loci < 32, 16, 48)
    mid32 = [same & (loc <= submid_f), same & (loc >= submid_b)]
    cm = np.zeros((128, 2, 4, 128), np.float32)
    for d in range(2):
        cm[:, d, 0] = tri[d].astype(np.float32) - mid[d].astype(np.float32)
        cm[:, d, 1] = tri[d].astype(np.float32)
        cm[:, d, 2] = allm.astype(np.float32) - tri[d].astype(np.float32)
        cm[:, d, 3] = tri[d].astype(np.float32) - mid32[d].astype(np.float32)
    c["cummat"] = cm
    same32 = (k // 32) == (i // 32)
    bd = np.zeros((128, 2, 128), np.float32)
    bd[:, 0] = (same32 & (k <= i)).astype(np.float32)
    bd[:, 1] = (same32 & (k >= i)).astype(np.float32)
    c["bdmask"] = bd
    tmk = np.zeros((128, 2, 2), np.float32)
    l1 = (np.arange(128) % 64)
    tmk[:, 0, 0] = (l1 >= 32); tmk[:, 0, 1] = (l1 < 32)
    tmk[:, 1, 0] = (l1 < 32); tmk[:, 1, 1] = (l1 >= 32)
    c["tmask"] = tmk
    c["same64"] = same.astype(np.float32)
    dl = np.zeros((32, 32, 64), np.float32)
    for q in range(32):
        dl[q, q, :] = 1.0
    c["delta32"] = dl
    c["ones"] = np.ones((128, 128), np.float32)
    jp = np.zeros((128, 2, 128), np.float32)
    jp[:, 0, :] = np.arange(1, 129, dtype=np.float32)[None]
    jp[:, 1, :] = (128 - np.arange(128, dtype=np.float32))[None]
    c["jp1"] = jp
    sg = np.ones((128, 2), np.float32)
    sg[64:, 0] = -1.0
    sg[:, 1] = -1.0
    c["sgn"] = sg
    pm = np.zeros((128, 2, 128), np.float32)
    for p in range(64):
        pm[64 + p, 0, p] = 1.0
        pm[p, 1, 64 + p] = 1.0
    c["pmat"] = pm
    sh = np.zeros((4, 2, 4, 128), np.float32)
    for h in range(4):
        sh[h, 0, h, :] = 1.0
        sh[h, 1, h, :] = -1.0
    c["selh"] = sh
    NEG = -30000.0
    nm = np.zeros((128, 2, 3, 128), np.float32)
    a_ = np.arange(128)[:, None]
    b_ = np.arange(128)[None, :]
    sm = (a_ // 64) == (b_ // 64)
    nm[:, 0, 0] = np.where(sm & (b_ < a_), 0.0, NEG)
    nm[:, 0, 1] = np.where(sm & (a_ < b_), 0.0, NEG)
    nm[:, 0, 2] = np.where(sm & (a_ <= b_), 0.0, NEG)
    nm[:, 1, 0] = np.where(sm & (b_ > a_), 0.0, NEG)
    nm[:, 1, 1] = np.where(sm & (a_ > b_), 0.0, NEG)
    nm[:, 1, 2] = np.where(sm & (a_ >= b_), 0.0, NEG)
    c["negmask"] = nm
    cmk = np.zeros((128, 2), np.float32)
    cmk[0:64, 0] = 1.0
    cmk[64:128, 1] = 1.0
    c["cmask"] = cmk
    t = np.arange(nlat)
    rows = (t // 64).astype(np.float32)
    cols = (t % 64).astype(np.float32)
    inv = (10000.0 ** (-np.arange(16, dtype=np.float32) / 16)).astype(np.float32)
    ang = np.concatenate([rows[:, None] * inv, cols[:, None] * inv], -1).astype(np.float32)
    cos = np.concatenate([np.ones((CTX, 32), np.float32), np.cos(ang).astype(np.float32)], 0)
    sin = np.concatenate([np.zeros((CTX, 32), np.float32), np.sin(ang).astype(np.float32)], 0)
    c["rot"] = np.stack([cos, sin], 1).astype(np.float32)
    sel = np.zeros((8, 8, 128), np.float32)
    for e in range(8):
        sel[e, e, :] = 1.0
    c["selrow8"] = sel
    return c


def prep_inputs(inp, b, nlat, depth):
    m = {}
    x = np.asarray(inp["x"][b], np.float32)[:nlat]
    ctx = np.asarray(inp["ctx"][b], np.float32)
    m["xT"] = np.ascontiguousarray(np.concatenate([ctx, x], 0).T)
    cv = np.stack([np.asarray(inp["c"][b], np.float32), np.asarray(inp["c_ctx"], np.float32)], -1)
    m["cvec"] = np.ascontiguousarray(cv.reshape(KT, 128, 2).transpose(1, 0, 2))
    m["ada_w"] = np.asarray(inp["ada_w"][:depth], np.float32)
    m["ada_b"] = np.ascontiguousarray(np.asarray(inp["ada_b"][:depth], np.float32).reshape(depth, 48, 128).transpose(2, 0, 1))
    w = np.asarray(inp["w_in"][:depth], np.float32)
    fm = np.concatenate([w[:, :, 0:768], w[:, :, 1040:1296]], -1)
    tm = np.concatenate([w[:, :, 768:1024], w[:, :, 1296:1552], w[:, :, 1552:2064], w[:, :, 2064:2320],
                         w[:, :, 2320:2576], w[:, :, 2576:3344], w[:, :, 3344:3600], w[:, :, 1024:1040]], -1)
    m["w_in"] = np.ascontiguousarray(np.concatenate([fm, tm], -1))
    m["w_out"] = np.asarray(inp["w_out"][:depth], np.float32)
    lng = np.asarray(inp["ln_g"][:depth], np.float32).reshape(depth, 2, KT, 128)
    lnb = np.asarray(inp["ln_b"][:depth], np.float32).reshape(depth, 2, KT, 128)
    m["ln_gb"] = np.ascontiguousarray(np.stack([lng, lnb], 0).transpose(4, 2, 3, 0, 1))
    m["ln_gb"] = np.ascontiguousarray(np.stack([lng, lnb], 2).transpose(4, 0, 1, 2, 3))
    nd = (depth + 1) // 2
    nm = depth // 2
    m["ffn_w1"] = np.asarray(inp["ffn_w1"][:nd], np.float32)
    m["ffn_w3"] = np.asarray(inp["ffn_w3"][:nd], np.float32)
    m["ffn_w2"] = np.asarray(inp["ffn_w2"][:nd], np.float32)
    if nm > 0:
        m["moe_router"] = np.asarray(inp["moe_router"][:nm], np.float32)
        m["moe_w1"] = np.asarray(inp["moe_w1"][:nm], np.float32)
        m["moe_w3"] = np.asarray(inp["moe_w3"][:nm], np.float32)
        m["moe_w2"] = np.asarray(inp["moe_w2"][:nm], np.float32)
    rep = lambda a: np.ascontiguousarray(np.broadcast_to(np.asarray(a, np.float32)[None], (128,) + tuple(np.shape(a))))
    m["hlb_rep"] = rep(inp["hgrn_lower_bounds"])
    m["hnw_rep"] = rep(inp["hgrn_norm_w"][:depth])
    m["gnw_rep"] = rep(inp["gdn_norm_w"][:depth])
    m["retdec_rep"] = rep(inp["ret_decay"][:depth])
    m["galog_rep"] = rep(np.asarray(inp["gdn_a_log"][:depth], np.float32).reshape(depth, 8))
    m["gdtb_rep"] = rep(np.asarray(inp["gdn_dt_bias"][:depth], np.float32).reshape(depth, 8))
    m["gconv_col"] = np.ascontiguousarray(np.asarray(inp["gdn_conv_w"][:depth], np.float32).reshape(depth, 3, 6, 128).transpose(3, 0, 1, 2))
    f32 = lambda a: np.asarray(a, np.float32)
    L = depth
    are = f32(inp["s5_a_re"][:L]).reshape(L, 32, 64)
    aim = f32(inp["s5_a_im"][:L]).reshape(L, 32, 64)
    lst = f32(inp["s5_log_step"][:L]).reshape(L, 32)
    m["s5_lam"] = np.ascontiguousarray(np.stack([are, aim], 2).transpose(1, 0, 2, 3))
    m["s5_lstep"] = np.ascontiguousarray(lst[:, :, None])
    dup = lambda a: np.concatenate([a, a], 0)
    m["s5_lamT"] = np.ascontiguousarray(dup(np.stack([are, aim], 1).transpose(3, 0, 1, 2)))
    m["s5_lstepT"] = np.ascontiguousarray(np.broadcast_to(lst[None], (128, L, 32)))
    bre = f32(inp["s5_b_re"][:L]).reshape(L, 32, 64, 16)
    bim = f32(inp["s5_b_im"][:L]).reshape(L, 32, 64, 16)
    BT = np.zeros((128, L, 2, 32, 64), np.float32)
    cre = f32(inp["s5_c_re"][:L]).reshape(L, 32, 16, 64)
    cim = f32(inp["s5_c_im"][:L]).reshape(L, 32, 16, 64)
    CT = np.zeros((128, L, 2, 32, 128), np.float32)
    for q in range(32):
        g = q % 16
        r0 = 16 * (g % 8)
        BT[r0:r0 + 16, :, 0, q, :] = bre[:, q].transpose(2, 0, 1)
        BT[r0:r0 + 16, :, 1, q, :] = bim[:, q].transpose(2, 0, 1)
        CT[0:64, :, 0, q, r0:r0 + 16] = cre[:, q].transpose(2, 0, 1)
        CT[64:128, :, 0, q, r0:r0 + 16] = cim[:, q].transpose(2, 0, 1)
        CT[0:64, :, 1, q, r0:r0 + 16] = cim[:, q].transpose(2, 0, 1)
        CT[64:128, :, 1, q, r0:r0 + 16] = cre[:, q].transpose(2, 0, 1)
    m["s5_BT"] = BT
    m["s5_CT"] = CT
    m["s5_dcol"] = np.ascontiguousarray(f32(inp["s5_d"][:L]).reshape(L, 2, 128).transpose(2, 0, 1))
    m["s5_gbcol"] = np.ascontiguousarray(f32(inp["s5_glu_b"][:L]).reshape(L, 2, 128).transpose(2, 0, 1))
    m["s5_glu_w"] = f32(inp["s5_glu_w"][:L])
    for k_, v in host_consts(nlat).items():
        m["c_" + k_] = v
    return m


def widths(total, maxw=512):
    out = []
    t = 0
    while t < total:
        w = min(maxw, total - t)
        out.append((t, w))
        t += w
    return out


class Builder:
    def __init__(self, nlat, depth, in_shapes, mixers=("gdn", "s5", "hgrn", "ret"), dbg=()):
        self.nlat = nlat
        self.depth = depth
        self.T = CTX + nlat
        self.NT = self.T // 128
        self.mixers = mixers
        self.dbg = dbg
        self.nc = bass.Bass("TRN2", target_bir_lowering=False)
        nc = self.nc
        self.din = {}
        for name, shp in in_shapes.items():
            self.din[name] = nc.dram_tensor(name, list(shp), F32, kind="ExternalInput").ap()
        self.outT = nc.dram_tensor("outT", [D, nlat], F32, kind="ExternalOutput").ap()
        self.dbg_out = {}

    def seg(self, t0, w):
        segs = []
        if t0 < CTX:
            segs.append((1, 0, min(w, CTX - t0)))
        if t0 + w > CTX:
            a = max(0, CTX - t0)
            segs.append((0, a, w))
        return segs

    def build(self):
        nc = self.nc
        with ExitStack() as es:
            S = Sched(nc, es)
            self.S = S
            self.es = es
            self.setup_global()
            for l in range(self.depth):
                self.layer(l)
            S.drain_all()
        return nc

    def setup_global(self):
        S = self.S
        T_ = self.T
        d = self.din
        self.X = [S.dram("Xa", [D, T_]), S.dram("Xb", [D, T_])]
        self.X1 = S.dram("X1", [D, T_])
        self.PF = S.dram("PF", [D, T_])
        self.PT = S.dram("PT", [T_, TMW])
        self.MIXT = S.dram("MIXT", [D, T_])
        self.xin = T(d["xT"], Res("xT"))
        self.ident = S.sb("ident", [128, 128])
        S.dma("sp", self.ident[:], d["c_ident"][:, :], writes=[self.ident])
        self.onesdiv = S.sb("onesdiv", [128, 128])
        S.dma("sp", self.onesdiv[:], d["c_onesdiv"][:, :], writes=[self.onesdiv])
        self.eps = S.sb("eps", [128, 2])
        S.memset("dve", self.eps[:, 0:1], LN_EPS, [self.eps])
        S.memset("dve", self.eps[:, 1:2], RMS_EPS, [self.eps])
        self.cvec = S.sb("cvec", [128, KT, 2])
        S.dma("sp", self.cvec[:], d["cvec"][:, :, :], writes=[self.cvec])
        self.csilu = S.sb("csilu", [128, KT, 2])
        S.act(self.csilu[:], self.cvec[:], AF.Silu, [self.cvec], [self.csilu])
        L = self.depth
        self.adab = S.sb("adab", [128, L, 48])
        S.dma("sp", self.adab[:], d["ada_b"][:, :, :], writes=[self.adab])
        self.lngb = S.sb("lngb", [128, L, 2, 2, KT])
        S.dma("sp", self.lngb[:], d["ln_gb"][:, :, :, :, :], writes=[self.lngb])
        self.MOD = S.sb("MOD", [128, 48, 2])
        self.OPS = S.sb("OPS", [128, 2, KT, 2])
        self.banks = [S.ps("bank%d" % i, [128, 512]) for i in range(8)]
        self.bankrot = Rot(self.banks)
        self.setup_gla_consts()

    def bank(self):
        return self.bankrot.next()

    def layer(self, l):
        self.phase_mod(l)
        self.S.barrier()
        self.phase_inproj(l)
        self.S.barrier()
        self.phase_mixers(l)
        self.S.barrier()
        self.phase_outproj(l)
        self.S.barrier()
        self.phase_ffn(l)
        self.S.barrier()

    def phase_mod(self, l):
        S = self.S
        d = self.din
        with ExitStack() as es:
            wbuf = [S.sb("adaw", [128, KT, 1024], es=es) for _ in range(2)]
            src = d["ada_w"][l].rearrange("(k p) c -> p k c", p=128)
            for jg in range(6):
                wb = wbuf[jg % 2]
                for k in range(KT):
                    S.dma("sp", wb[:, k, :], src[:, k, jg * 1024:(jg + 1) * 1024], writes=[wb])
                for j in range(8):
                    ps = self.bank()
                    for k in range(KT):
                        S.mm(ps[:, 0:2], wb[:, k, j * 128:(j + 1) * 128], self.csilu[:, k, :],
                             k == 0, k == KT - 1, [wb, self.csilu], [ps])
                    S.tt("dve", self.MOD[:, jg * 8 + j, :], ps[:, 0:2],
                         self.adab[:, l, jg * 8 + j:jg * 8 + j + 1].to_broadcast([128, 2]), ALU.add,
                         [ps, self.adab, wb], [self.MOD])
            S.ts("dve", self.OPS[:, 0, :, :], self.MOD[:, 8:16, :], 1.0, ALU.add, [self.MOD], [self.OPS])
            S.ts("dve", self.OPS[:, 1, :, :], self.MOD[:, 32:40, :], 1.0, ALU.add, [self.MOD], [self.OPS])
        if "MOD" in self.dbg and l == 0:
            o = self.nc.dram_tensor("dbg_MOD", [128, 48, 2], F32, kind="ExternalOutput").ap()
            S.dma("sp", o, self.MOD[:], reads=[self.MOD])

    def load_w_bf16(self, dst, src_ap, nk):
        S = self.S
        v = src_ap.rearrange("(k p) c -> p k c", p=128)
        for k in range(nk):
            S.dma("pool", dst[:, k, :], v[:, k, :], writes=[dst])

    def phase_inproj(self, l):
        S = self.S
        d = self.din
        xsrc = self.xin if l == 0 else self.X[l % 2]
        with ExitStack() as es:
            win = S.sb("win", [128, KT, DPROJ], BF16, es=es)
            self.load_w_bf16(win, d["w_in"][l], KT)
            xb = Rot([S.sb("xblk", [128, KT, 512], es=es) for _ in range(2)])
            xmr = Rot([S.sb("xm", [128, KT, 512], BF16, es=es) for _ in range(2)])
            stg = Rot([S.sb("stg", [128, 512], es=es) for _ in range(4)])
            evq = Rot(["dve", "act"])
            xv = xsrc.t.rearrange("(k p) t -> p k t", p=128)
            for (t0, W) in widths(self.T):
                xblk = xb.next()
                xm = xmr.next()
                for k in range(KT):
                    S.dma("sp", xblk[:, k, :W], xv[:, k, t0:t0 + W], reads=[xsrc], writes=[xblk])
                for k in range(KT):
                    for (s, a, b) in self.seg(t0, W):
                        S.act(xm[:, k, a:b], xblk[:, k, a:b], AF.Identity, [xblk, self.OPS, self.MOD], [xm],
                              scale=self.OPS[:, 0, k, s:s + 1], bias=self.MOD[:, 0 + k, s:s + 1])
                for m in range(8):
                    ps = self.bank()
                    for k in range(KT):
                        S.mm(ps[:, :W], win[:, k, m * 128:(m + 1) * 128], xm[:, k, :W], k == 0, k == KT - 1,
                             [win, xm], [ps])
                    st = stg.next()
                    S.copy(evq.next(), st[:, :W], ps[:, :W], [ps], [st])
                    S.dma("sp", self.PF[m * 128:(m + 1) * 128, t0:t0 + W], st[:, :W], reads=[st], writes=[self.PF])
                for sub in range(W // 128):
                    for (c0, cw) in TMGROUPS:
                        ps = self.bank()
                        for k in range(KT):
                            S.mm(ps[:, :cw], xm[:, k, sub * 128:(sub + 1) * 128], win[:, k, 1024 + c0:1024 + c0 + cw],
                                 k == 0, k == KT - 1, [win, xm], [ps])
                        st = stg.next()
                        S.copy(evq.next(), st[:, :cw], ps[:, :cw], [ps], [st])
                        S.dma("sp", self.PT[t0 + sub * 128:t0 + (sub + 1) * 128, c0:c0 + cw], st[:, :cw],
                              reads=[st], writes=[self.PT])
        if "PF" in self.dbg and l == 0:
            self.dump("PF", self.PF, [D, self.T])
            self.dump("PT", self.PT, [self.T, TMW])

    def dump(self, name, src, shape):
        S = self.S
        o = self.nc.dram_tensor("dbg_" + name, list(shape), F32, kind="ExternalOutput").ap()
        self.dbg_out[name] = o
        S.dma("sp", o, src.t, reads=[src])

    def phase_mixers(self, l):
        S = self.S
        order = ["gdn", "s5", "hgrn", "ret"]
        with ExitStack() as es:
            z = S.sb("zeros", [128, 512], es=es)
            S.memset("dve", z[:], 0.0, [z])
            for mi, name in enumerate(order):
                if name in self.mixers:
                    continue
                for r in range(2):
                    for (t0, W) in widths(self.T):
                        S.dma("sp", self.MIXT[mi * 256 + r * 128: mi * 256 + (r + 1) * 128, t0:t0 + W], z[:, :W],
                              reads=[z], writes=[self.MIXT])
            S.barrier()
        if "hgrn" in self.mixers:
            self.mixer_gla(l, "hgrn")
        if "ret" in self.mixers:
            self.mixer_gla(l, "ret")
        if "s5" in self.mixers:
            self.mixer_s5(l)
        if "gdn" in self.mixers:
            self.mixer_gdn(l)
        if "MIXT" in self.dbg and l == 0:
            self.dump("MIXT", self.MIXT, [D, self.T])

    def ln_block(self, es_tiles, y, W, l, sub, out, extra=None):
        S = self.S
        sq, mean, rstd, tmp = es_tiles["sq"], es_tiles["mean"], es_tiles["rstd"], es_tiles["tmp"]
        p1 = self.bank()
        p2 = self.bank()
        for m in range(KT):
            S.mm(p1[:, :W], self.onesdiv[:], y[:, m, :W], m == 0, m == KT - 1, [self.onesdiv, y], [p1])
        for m in range(KT):
            sqm = sq.next()
            S.act(sqm[:, :W], y[:, m, :W], AF.Square, [y], [sqm])
            S.mm(p2[:, :W], self.onesdiv[:], sqm[:, :W], m == 0, m == KT - 1, [self.onesdiv, sqm], [p2])
        S.copy("act", mean[:, :W], p1[:, :W], [p1], [mean])
        S.tt("dve", rstd[:, :W], mean[:, :W], mean[:, :W], ALU.mult, [mean], [rstd])
        S.tt("dve", rstd[:, :W], p2[:, :W], rstd[:, :W], ALU.subtract, [p2, rstd], [rstd])
        S.act(rstd[:, :W], rstd[:, :W], AF.Ln, [rstd, self.eps], [rstd], bias=self.eps[:, 0:1], scale=1.0)
        S.act(rstd[:, :W], rstd[:, :W], AF.Exp, [rstd], [rstd], scale=-0.5)
        for m in range(KT):
            tm_ = tmp.next()
            S.tt("dve", tm_[:, :W], y[:, m, :W], mean[:, :W], ALU.subtract, [y, mean], [tm_])
            S.tt("pool", tm_[:, :W], tm_[:, :W], rstd[:, :W], ALU.mult, [tm_, rstd], [tm_])
            S.act(out[:, m, :W], tm_[:, :W], AF.Identity, [tm_, self.lngb], [out],
                  scale=self.lngb[:, l, sub, 0, m:m + 1], bias=self.lngb[:, l, sub, 1, m:m + 1])

    def phase_outproj(self, l):
        S = self.S
        d = self.din
        xsrc = self.xin if l == 0 else self.X[l % 2]
        with ExitStack() as es:
            wout = S.sb("wout", [128, KT, D], BF16, es=es)
            self.load_w_bf16(wout, d["w_out"][l], KT)
            xb = Rot([S.sb("xblk", [128, KT, 512], es=es) for _ in range(2)])
            mb = Rot([S.sb("mixb", [128, KT, 512], BF16, es=es) for _ in range(2)])
            yb = Rot([S.sb("yb", [128, KT, 512], es=es) for _ in range(2)])
            ob = Rot([S.sb("ob", [128, KT, 512], es=es) for _ in range(2)])
            lt = {"sq": Rot([S.sb("sq", [128, 512], es=es) for _ in range(3)]), "mean": S.sb("mean", [128, 512], es=es),
                  "rstd": S.sb("rstd", [128, 512], es=es),
                  "tmp": Rot([S.sb("lntmp", [128, 512], es=es) for _ in range(3)])}
            xv = xsrc.t.rearrange("(k p) t -> p k t", p=128)
            mv = self.MIXT.t.rearrange("(k p) t -> p k t", p=128)
            x1v = self.X1.t.rearrange("(k p) t -> p k t", p=128)
            for (t0, W) in widths(self.T):
                xblk, mixb, y, o = xb.next(), mb.next(), yb.next(), ob.next()
                for k in range(KT):
                    S.dma("sp", xblk[:, k, :W], xv[:, k, t0:t0 + W], reads=[xsrc], writes=[xblk])
                    S.dma("pool", mixb[:, k, :W], mv[:, k, t0:t0 + W], reads=[self.MIXT], writes=[mixb])
                for k in range(KT):
                    S.op("act", lambda e, k=k: e.mul(out=xblk[:, k, :W], in_=xblk[:, k, :W], mul=ALPHA), [xblk], [xblk])
                for m in range(KT):
                    ps = self.bank()
                    for k in range(KT):
                        S.mm(ps[:, :W], wout[:, k, m * 128:(m + 1) * 128], mixb[:, k, :W], k == 0, k == KT - 1,
                             [wout, mixb], [ps])
                    for (s, a, b) in self.seg(t0, W):
                        S.stt("dve", y[:, m, a:b], ps[:, a:b], self.MOD[:, 16 + m, s:s + 1], xblk[:, m, a:b],
                              ALU.mult, ALU.add, [ps, self.MOD, xblk], [y])
                self.ln_block(lt, y, W, l, 0, o)
                for k in range(KT):
                    S.dma("sp", x1v[:, k, t0:t0 + W], o[:, k, :W], reads=[o], writes=[self.X1])
        if "X1" in self.dbg and l == 0:
            self.dump("X1", self.X1, [D, self.T])

    def phase_ffn(self, l):
        S = self.S
        d = self.din
        moe = (l % 2 == 1)
        j = l // 2
        F = FF_EXPERT if moe else FF_DENSE
        nexp = NEXP if moe else 1
        last = (l == self.depth - 1)
        xdst = self.X[(l + 1) % 2]
        TB = 1408 if self.T % 1408 == 0 else self.T
        assert self.T % TB == 0 and TB <= 1408
        fgroups = widths(F // 128, 4)
        with ExitStack() as es:
            x1b = Rot([S.sb("x1b", [128, KT, 512], es=es) for _ in range(2)])
            xm = S.sb("xm2", [128, KT, TB], BF16, es=es)
            acc = S.sb("acc", [128, KT, TB], es=es)
            w1r = Rot([S.sb("w1g", [128, KT, 512], BF16, es=es) for _ in range(2)])
            w3r = Rot([S.sb("w3g", [128, KT, 512], BF16, es=es) for _ in range(2)])
            w2r = Rot([S.sb("w2g", [128, 4, D], BF16, es=es) for _ in range(2)])
            gr = Rot([S.sb("gact", [128, 4, 512], BF16, es=es) for _ in range(2)])
            s1r = Rot([S.sb("s1", [128, 512], es=es) for _ in range(3)])
            lt = {"sq": Rot([S.sb("sq", [128, 512], es=es) for _ in range(3)]), "mean": S.sb("mean", [128, 512], es=es),
                  "rstd": S.sb("rstd", [128, 512], es=es),
                  "tmp": Rot([S.sb("lntmp", [128, 512], es=es) for _ in range(3)])}
            if moe:
                router = S.sb("router", [128, KT, 8], es=es)
                S.dma("sp", router[:], d["moe_router"][j].rearrange("(k p) e -> p k e", p=128), writes=[router])
                selrow = S.sb("selrow", [8, 8, 128], es=es)
                S.dma("sp", selrow[:], d["c_selrow8"][:, :, :], writes=[selrow])
                GT = S.sb("GT", [8, TB], es=es)
                xf = S.sb("xf", [128, KT, 128], es=es)
                rt = {n: S.sb("rt_" + n, [128, 8], es=es) for n in ("lg", "eq", "lg2", "sel", "e", "g")}
                rc = {n: S.sb("rc_" + n, [128, 1], es=es) for n in ("m1", "m2", "nm1", "den")}
                gbc = Rot([S.sb("gbc", [128, 512], es=es) for _ in range(2)])
            x1v = self.X1.t.rearrange("(k p) t -> p k t", p=128)
            xdv = xdst.t.rearrange("(k p) t -> p k t", p=128)
            for sb0 in range(0, self.T, TB):
                for k in range(KT):
                    S.memset("pool", acc[:, k, :], 0.0, [acc])
                for (c0, W) in widths(TB):
                    x1 = x1b.next()
                    for k in range(KT):
                        S.dma("sp", x1[:, k, :W], x1v[:, k, sb0 + c0:sb0 + c0 + W], reads=[self.X1], writes=[x1])
                    for k in range(KT):
                        for (s, a, b) in self.seg(sb0 + c0, W):
                            S.act(xm[:, k, c0 + a:c0 + b], x1[:, k, a:b], AF.Identity, [x1, self.OPS, self.MOD], [xm],
                                  scale=self.OPS[:, 1, k, s:s + 1], bias=self.MOD[:, 24 + k, s:s + 1])
                    if not moe:
                        continue
                    for sub in range(W // 128):
                        q0 = sub * 128
                        s = 1 if (sb0 + c0 + q0) < CTX else 0
                        for k in range(KT):
                            S.act(xf[:, k, :], x1[:, k, q0:q0 + 128], AF.Identity, [x1, self.OPS, self.MOD], [xf],
                                  scale=self.OPS[:, 1, k, s:s + 1], bias=self.MOD[:, 24 + k, s:s + 1])
                        ps = self.bank()
                        for k in range(KT):
                            S.mm(ps[:, 0:8], xf[:, k, :], router[:, k, :], k == 0, k == KT - 1, [xf, router], [ps])
                        lg, eq, lg2, sel, ee, gg = (rt[n] for n in ("lg", "eq", "lg2", "sel", "e", "g"))
                        S.copy("dve", lg[:], ps[:, 0:8], [ps], [lg])
                        S.op("dve", lambda e: e.reduce_max(out=rc["m1"][:], in_=lg[:], axis=AX.X), [lg], [rc["m1"]])
                        S.ts("dve", eq[:], lg[:], rc["m1"][:, 0:1], ALU.is_equal, [lg, rc["m1"]], [eq])
                        S.stt("dve", lg2[:], eq[:], -1e30, lg[:], ALU.mult, ALU.add, [eq, lg], [lg2])
                        S.op("dve", lambda e: e.reduce_max(out=rc["m2"][:], in_=lg2[:], axis=AX.X), [lg2], [rc["m2"]])
                        S.ts("dve", sel[:], lg[:], rc["m2"][:, 0:1], ALU.is_ge, [lg, rc["m2"]], [sel])
                        S.ts("dve", rc["nm1"][:], rc["m1"][:], -1.0, ALU.mult, [rc["m1"]], [rc["nm1"]])
                        S.act(ee[:], lg[:], AF.Exp, [lg, rc["nm1"]], [ee], bias=rc["nm1"][:, 0:1], scale=1.0)
                        S.tt("dve", ee[:], ee[:], sel[:], ALU.mult, [ee, sel], [ee])
                        S.op("dve", lambda e: e.reduce_sum(out=rc["den"][:], in_=ee[:], axis=AX.X), [ee], [rc["den"]])
                        S.op("dve", lambda e: e.reciprocal(out=rc["den"][:], in_=rc["den"][:]), [rc["den"]], [rc["den"]])
                        S.ts("dve", gg[:], ee[:], rc["den"][:, 0:1], ALU.mult, [ee, rc["den"]], [gg])
                        pt = self.bank()
                        S.tr(pt[0:8, 0:128], gg[:], self.ident[:], [gg, self.ident], [pt])
                        S.copy("dve", GT[:, c0 + q0:c0 + q0 + 128], pt[0:8, 0:128], [pt], [GT])
                for e_ in range(nexp):
                    if moe:
                        w1s, w3s, w2s = d["moe_w1"][j, e_], d["moe_w3"][j, e_], d["moe_w2"][j, e_]
                    else:
                        w1s, w3s, w2s = d["ffn_w1"][j], d["ffn_w3"][j], d["ffn_w2"][j]
                    w1v = w1s.rearrange("(k p) f -> p k f", p=128)
                    w3v = w3s.rearrange("(k p) f -> p k f", p=128)
                    w2v = w2s.rearrange("(fi p) c -> p fi c", p=128)
                    for (f0, nf) in fgroups:
                        w1g, w3g, w2g = w1r.next(), w3r.next(), w2r.next()
                        for k in range(KT):
                            S.dma("pool", w1g[:, k, :nf * 128], w1v[:, k, f0 * 128:(f0 + nf) * 128], writes=[w1g])
                            S.dma("pool", w3g[:, k, :nf * 128], w3v[:, k, f0 * 128:(f0 + nf) * 128], writes=[w3g])
                        S.dma("pool", w2g[:, :nf, :], w2v[:, f0:f0 + nf, :], writes=[w2g])
                        for (c0, W) in widths(TB):
                            if moe:
                                gb = gbc.next()
                                pg = self.bank()
                                S.mm(pg[:, :W], selrow[:, e_, :], GT[:, c0:c0 + W], True, True, [selrow, GT], [pg])
                                S.copy("act", gb[:, :W], pg[:, :W], [pg], [gb])
                            g = gr.next()
                            for fi in range(nf):
                                p1 = self.bank()
                                p3 = self.bank()
                                for k in range(KT):
                                    S.mm(p1[:, :W], w1g[:, k, fi * 128:(fi + 1) * 128], xm[:, k, c0:c0 + W],
                                         k == 0, k == KT - 1, [w1g, xm], [p1])
                                for k in range(KT):
                                    S.mm(p3[:, :W], w3g[:, k, fi * 128:(fi + 1) * 128], xm[:, k, c0:c0 + W],
                                         k == 0, k == KT - 1, [w3g, xm], [p3])
                                s1 = s1r.next()
                                S.act(s1[:, :W], p1[:, :W], AF.Silu, [p1], [s1])
                                if moe:
                                    S.tt("dve", s1[:, :W], s1[:, :W], p3[:, :W], ALU.mult, [s1, p3], [s1])
                                    S.tt("pool", g[:, fi, :W], s1[:, :W], gb[:, :W], ALU.mult, [s1, gb], [g])
                                else:
                                    S.tt("dve", g[:, fi, :W], s1[:, :W], p3[:, :W], ALU.mult, [s1, p3], [g])
                            for m in range(KT):
                                po = self.bank()
                                for fi in range(nf):
                                    S.mm(po[:, :W], w2g[:, fi, m * 128:(m + 1) * 128], g[:, fi, :W], fi == 0, fi == nf - 1,
                                         [w2g, g], [po])
                                S.tt("dve", acc[:, m, c0:c0 + W], acc[:, m, c0:c0 + W], po[:, :W], ALU.add,
                                     [acc, po], [acc])
                for (c0, W) in widths(TB):
                    x1 = x1b.next()
                    t0 = sb0 + c0
                    for k in range(KT):
                        S.dma("sp", x1[:, k, :W], x1v[:, k, t0:t0 + W], reads=[self.X1], writes=[x1])
                    for m in range(KT):
                        S.op("act", lambda e, m=m, W=W, x1=x1: e.mul(out=x1[:, m, :W], in_=x1[:, m, :W], mul=ALPHA), [x1], [x1])
                        for (s, a, b) in self.seg(t0, W):
                            S.stt("dve", acc[:, m, c0 + a:c0 + b], acc[:, m, c0 + a:c0 + b], self.MOD[:, 40 + m, s:s + 1],
                                  x1[:, m, a:b], ALU.mult, ALU.add, [acc, self.MOD, x1], [acc])
                    yv = T(acc.t[:, :, c0:c0 + W], acc.res)
                    self.ln_block(lt, yv, W, l, 1, x1)
                    for k in range(KT):
                        if not last:
                            S.dma("sp", xdv[:, k, t0:t0 + W], x1[:, k, :W], reads=[x1], writes=[xdst])
                        else:
                            a = max(t0, CTX)
                            if a < t0 + W:
                                S.dma("sp", self.outT.rearrange("(k p) t -> p k t", p=128)[:, k, a - CTX:t0 + W - CTX],
                                      x1[:, k, (a - t0):W], reads=[x1])


    def setup_gla_consts(self):
        S = self.S
        d = self.din
        self.cummat = S.sb("cummat", [128, 2, 4, 128])
        S.dma("sp", self.cummat[:], d["c_cummat"][:, :, :, :], writes=[self.cummat])
        self.bdmask = S.sb("bdmask", [128, 2, 128])
        S.dma("sp", self.bdmask[:], d["c_bdmask"][:, :, :], writes=[self.bdmask])
        self.same64 = S.sb("same64", [128, 128])
        S.dma("sp", self.same64[:], d["c_same64"][:, :], writes=[self.same64])
        self.tmask = S.sb("tmask", [128, 2, 2])
        S.dma("sp", self.tmask[:], d["c_tmask"][:, :, :], writes=[self.tmask])
        self.LBALL = S.sb("LBALL", [128, 4, 256])
        with ExitStack() as es:
            raw = S.sb("lbraw", [128, 4, 256], es=es)
            mx = S.sb("lbmx", [128, 256], es=es)
            sm = S.sb("lbsm", [128, 256], es=es)
            S.dma("sp", raw[:], d["hlb_rep"][:, :, :], writes=[raw])
            S.tt("dve", mx[:], raw[:, 0, :], raw[:, 1, :], ALU.max, [raw], [mx])
            S.tt("dve", mx[:], mx[:], raw[:, 2, :], ALU.max, [raw, mx], [mx])
            S.tt("dve", mx[:], mx[:], raw[:, 3, :], ALU.max, [raw, mx], [mx])
            for i in range(4):
                S.tt("dve", raw[:, i, :], raw[:, i, :], mx[:], ALU.subtract, [raw, mx], [raw])
            S.act(raw[:], raw[:], AF.Exp, [raw], [raw])
            S.tt("dve", sm[:], raw[:, 0, :], raw[:, 1, :], ALU.add, [raw], [sm])
            S.tt("dve", sm[:], sm[:], raw[:, 2, :], ALU.add, [raw, sm], [sm])
            S.tt("dve", sm[:], sm[:], raw[:, 3, :], ALU.add, [raw, sm], [sm])
            S.op("dve", lambda e: e.reciprocal(out=sm[:], in_=sm[:]), [sm], [sm])
            for i in range(4):
                S.tt("dve", raw[:, i, :], raw[:, i, :], sm[:], ALU.mult, [raw, sm], [raw])
            S.memset("dve", self.LBALL[:, 0, :], 0.0, [self.LBALL])
            for i in range(1, 4):
                S.tt("dve", self.LBALL[:, i, :], self.LBALL[:, i - 1, :], raw[:, i, :], ALU.add, [self.LBALL, raw], [self.LBALL])
            S.barrier()

    def gla_decay(self, es, logf, dd, bank3):
        S = self.S
        outs = []
        for i in range(4):
            ps = bank3[i]
            S.mm(ps[:, 0:256], self.cummat[:, dd, i, :], logf[0], True, True, [self.cummat] + logf[1], [ps])
            outs.append(ps)
        return outs

    def mixer_gla(self, l, which):
        S = self.S
        d = self.din
        T_ = self.T
        NT = self.NT
        mrow = 512 if which == "hgrn" else 768
        Od = [S.dram("O_%s_f" % which, [T_, 256]), S.dram("O_%s_b" % which, [T_, 256])]
        with ExitStack() as es:
            if which == "hgrn":
                LB = T(self.LBALL.t[:, l, :], self.LBALL.res)
                OMLB = S.sb("omlb", [128, 256], es=es)
                S.ts("dve", OMLB[:], LB[:], -1.0, ALU.mult, [LB], [OMLB], s2=1.0, op1=ALU.add)
                NW = S.sb("nw", [128, 64], es=es)
                S.dma("sp", NW[:], d["hnw_rep"][:, l, :], writes=[NW])
            else:
                LG = S.sb("lg", [128, 2, 4], es=es)
                S.dma("sp", LG[:], d["retdec_rep"][:, l, :, :], writes=[LG])
                S.act(LG[:], LG[:], AF.Exp, [LG], [LG])
                S.ts("dve", LG[:], LG[:], -1.0, ALU.mult, [LG], [LG])
                LF = S.sb("lf", [128, 2, 256], es=es)
                for dd in range(2):
                    for h in range(4):
                        S.copy("dve", LF[:, dd, h * 64:(h + 1) * 64], LG[:, dd, h:h + 1].to_broadcast([128, 64]), [LG], [LF])
                CE = S.sb("ce", [128, 2, 6, 256], es=es)
                CEgT = S.sb("cegt", [128, 2, 2, 128], es=es)
                for dd in range(2):
                    b3 = [self.bank() for _ in range(4)]
                    A = self.gla_decay(es, (LF[:, dd, :], [LF]), dd, b3)
                    S.act(CE[:, dd, 0, :], A[0][:, 0:256], AF.Exp, [A[0]], [CE])
                    S.act(CE[:, dd, 1, :], A[0][:, 0:256], AF.Exp, [A[0]], [CE], scale=-1.0)
                    S.act(CE[:, dd, 2, :], A[1][:, 0:256], AF.Exp, [A[1]], [CE])
                    S.act(CE[:, dd, 3, :], A[2][:, 0:256], AF.Exp, [A[2]], [CE])
                    S.act(CE[:, dd, 4, :], A[3][:, 0:256], AF.Exp, [A[3]], [CE])
                    S.act(CE[:, dd, 5, :], A[3][:, 0:256], AF.Exp, [A[3]], [CE], scale=-1.0)
                    for ii in (1, 3, 5):
                        S.ts("dve", CE[:, dd, ii, :], CE[:, dd, ii, :], 0.125, ALU.mult, [CE], [CE])
                    pt = self.bank()
                    for p in range(2):
                        S.tr(pt[:, p * 128:(p + 1) * 128], CE[:, dd, 2, p * 128:(p + 1) * 128], self.ident[:], [CE, self.ident], [pt])
                    S.copy("dve", CEgT[:, dd, :, :], pt[:, 0:256].rearrange("p (a b) -> p a b", a=2), [pt], [CEgT])
            inq = Rot([S.sb("inq", [128, 512], es=es) for _ in range(2)])
            inf = Rot([S.sb("inf", [128, 256], es=es) for _ in range(2)])
            inv = Rot([S.sb("inv", [128, 256], es=es) for _ in range(2)])
            rot = Rot([S.sb("rot", [128, 2, 32], es=es) for _ in range(2)])
            qk = Rot([S.sb("qk", [128, 512], es=es) for _ in range(2)])
            rtmp = [S.sb("rtmp%d" % i, [128, 8, 32], es=es) for i in range(2)]
            ftile = Rot([S.sb("ftile", [128, 256], es=es) for _ in range(2)])
            lftile = Rot([S.sb("lftile", [128, 256], es=es) for _ in range(2)])
            ktile = Rot([S.sb("ktile", [128, 256], es=es) for _ in range(2)])
            Et = Rot([S.sb("Et", [128, 6, 256], es=es) for _ in range(2)])
            pre = Rot([S.sb("pre", [128, 6, 256], es=es) for _ in range(2)])
            preT = Rot([S.sb("preT", [128, 6, 2, 128], es=es) for _ in range(2)])
            EgT = Rot([S.sb("EgT", [128, 2, 128], es=es) for _ in range(2)])
            attm = Rot([S.sb("attm", [128, 128], es=es) for _ in range(12)])
            osb = Rot([S.sb("osb", [64, 128], es=es) for _ in range(4)])
            Sblk = [S.sb("Sblk%d" % p, [128, 128], es=es) for p in range(2)]
            for dd in range(2):
                for p in range(2):
                    S.memset("dve", Sblk[p][:], 0.0, [Sblk[p]])
                tiles = list(range(NT)) if dd == 0 else [1, 0] + list(range(NT - 1, 1, -1))
                chunks = [0, 1] if dd == 0 else [1, 0]
                for n in tiles:
                    t0 = n * 128
                    v = inv.next()
                    q_ap = None
                    if which == "hgrn":
                        iq, if_ = inq.next(), inf.next()
                        S.dma("sp", iq[:, 0:256], self.PT[t0:t0 + 128, TM_HQ:TM_HQ + 256], reads=[self.PT], writes=[iq])
                        S.dma("sp", if_[:], self.PT[t0:t0 + 128, TM_HF + dd * 256:TM_HF + (dd + 1) * 256], reads=[self.PT], writes=[if_])
                        S.dma("sp", v[:], self.PT[t0:t0 + 128, TM_HI:TM_HI + 256], reads=[self.PT], writes=[v])
                        qkt = qk.next()
                        S.act(qkt[:, 0:256], iq[:, 0:256], AF.Silu, [iq], [qkt])
                        ft, lft, kt_ = ftile.next(), lftile.next(), ktile.next()
                        S.act(ft[:], if_[:], AF.Sigmoid, [if_], [ft])
                        S.tt("dve", ft[:], ft[:], OMLB[:], ALU.mult, [ft, OMLB], [ft])
                        S.tt("dve", ft[:], ft[:], LB[:], ALU.add, [ft, LB], [ft])
                        S.act(lft[:], ft[:], AF.Ln, [ft], [lft])
                        S.ts("dve", kt_[:], ft[:], -1.0, ALU.mult, [ft], [kt_], s2=1.0, op1=ALU.add)
                        b3 = [self.bank() for _ in range(4)]
                        A = self.gla_decay(es, (lft[:], [lft]), dd, b3)
                        E = Et.next()
                        S.act(E[:, 0, :], A[0][:, 0:256], AF.Exp, [A[0]], [E])
                        S.act(E[:, 1, :], A[0][:, 0:256], AF.Exp, [A[0]], [E], scale=-1.0)
                        S.act(E[:, 2, :], A[1][:, 0:256], AF.Exp, [A[1]], [E])
                        S.act(E[:, 3, :], A[2][:, 0:256], AF.Exp, [A[2]], [E])
                        S.act(E[:, 4, :], A[3][:, 0:256], AF.Exp, [A[3]], [E])
                        S.act(E[:, 5, :], A[3][:, 0:256], AF.Exp, [A[3]], [E], scale=-1.0)
                        q_src, k_src = (qkt[:, 0:256], qkt), (kt_[:], kt_)
                        Eaps = [(E[:, i, :], E) for i in range(6)]
                        egt = EgT.next()
                        pt = self.bank()
                        for p in range(2):
                            S.tr(pt[:, p * 128:(p + 1) * 128], E[:, 2, p * 128:(p + 1) * 128], self.ident[:], [E, self.ident], [pt])
                        S.copy("act", egt[:], pt[:, 0:256].rearrange("p (a b) -> p a b", a=2), [pt], [egt])
                        egt_ap = lambda p_, c0_, egt=egt: (egt[:, p_, c0_:c0_ + 1], egt)
                    else:
                        iq = inq.next()
                        S.dma("sp", iq[:], self.PT[t0:t0 + 128, TM_RQ:TM_RQ + 512], reads=[self.PT], writes=[iq])
                        S.dma("sp", v[:], self.PT[t0:t0 + 128, TM_RV:TM_RV + 256], reads=[self.PT], writes=[v])
                        rt_ = rot.next()
                        S.dma("sp", rt_[:], d["c_rot"][t0:t0 + 128, :, :], writes=[rt_])
                        qkt = qk.next()
                        x3 = iq[:].rearrange("p (h e) -> p h e", e=64)
                        o3 = qkt[:].rearrange("p (h e) -> p h e", e=64)
                        cosb = rt_[:, 0, :].unsqueeze(1).to_broadcast([128, 8, 32])
                        sinb = rt_[:, 1, :].unsqueeze(1).to_broadcast([128, 8, 32])
                        a_, b_ = rtmp
                        S.tt("dve", a_[:], x3[:, :, 0:32], cosb, ALU.mult, [iq, rt_], [a_])
                        S.tt("pool", b_[:], x3[:, :, 32:64], sinb, ALU.mult, [iq, rt_], [b_])
                        S.tt("dve", o3[:, :, 0:32], a_[:], b_[:], ALU.subtract, [a_, b_], [qkt])
                        S.tt("dve", a_[:], x3[:, :, 0:32], sinb, ALU.mult, [iq, rt_], [a_])
                        S.tt("pool", b_[:], x3[:, :, 32:64], cosb, ALU.mult, [iq, rt_], [b_])
                        S.tt("dve", o3[:, :, 32:64], a_[:], b_[:], ALU.add, [a_, b_], [qkt])
                        q_src, k_src = (qkt[:, 0:256], qkt), (qkt[:, 256:512], qkt)
                        Eaps = [(CE[:, dd, i, :], CE) for i in range(6)]
                        egt_ap = lambda p_, c0_, dd=dd: (CEgT[:, dd, p_, c0_:c0_ + 1], CEgT)
                    pr = pre.next()
                    tm = self.tmask
                    S.stt("dve", pr[:, 0, :], q_src[0], tm[:, dd, 0:1], Eaps[0][0], ALU.mult, ALU.mult, [q_src[1], Eaps[0][1], tm], [pr])
                    S.stt("dve", pr[:, 1, :], k_src[0], tm[:, dd, 1:2], Eaps[1][0], ALU.mult, ALU.mult, [k_src[1], Eaps[1][1], tm], [pr])
                    S.tt("dve", pr[:, 2, :], q_src[0], Eaps[2][0], ALU.mult, [q_src[1], Eaps[2][1]], [pr])
                    S.tt("pool", pr[:, 3, :], k_src[0], Eaps[3][0], ALU.mult, [k_src[1], Eaps[3][1]], [pr])
                    S.tt("dve", pr[:, 4, :], q_src[0], Eaps[4][0], ALU.mult, [q_src[1], Eaps[4][1]], [pr])
                    S.tt("pool", pr[:, 5, :], k_src[0], Eaps[5][0], ALU.mult, [k_src[1], Eaps[5][1]], [pr])
                    prT = preT.next()
                    for ii, i in enumerate((0, 1, 2, 4, 5)):
                        pt = self.bank()
                        for p in range(2):
                            S.tr(pt[:, p * 128:(p + 1) * 128], pr[:, i, p * 128:(p + 1) * 128], self.ident[:], [pr, self.ident], [pt])
                        S.copy("act" if ii % 2 else "dve", prT[:, i, :, :], pt[:, 0:256].rearrange("p (a b) -> p a b", a=2), [pt], [prT])
                    ams = []
                    for h in range(4):
                        p, hh = h // 2, h % 2
                        hs_ = slice(64 * hh, 64 * hh + 64)
                        psd = self.bank()
                        S.mm(psd[:, 0:128], prT[hs_, 5, p, :], prT[hs_, 4, p, :], True, True, [prT], [psd])
                        pso = self.bank()
                        S.mm(pso[:, 0:128], prT[hs_, 1, p, :], prT[hs_, 0, p, :], True, True, [prT], [pso])
                        am = attm.next()
                        am2 = attm.next()
                        S.tt("dve", am[:], psd[:, 0:128], self.bdmask[:, dd, :], ALU.mult, [psd, self.bdmask], [am])
                        S.tt("dve", am2[:], pso[:, 0:128], self.same64[:], ALU.mult, [pso, self.same64], [am2])
                        S.tt("pool", am[:], am[:], am2[:], ALU.add, [am, am2], [am])
                        ams.append(am)
                    for c in chunks:
                        cs = slice(c * 64, (c + 1) * 64)
                        ecol = c * 64 + (63 if dd == 0 else 0)
                        for p in range(2):
                            po = self.bank()
                            S.mm(po[0:64, 0:128], prT[:, 2, p, cs], Sblk[p][:], True, False, [prT, Sblk[p]], [po])
                            for hh in range(2):
                                h = 2 * p + hh
                                S.mm(po[0:64, hh * 64:(hh + 1) * 64], ams[h][:, cs], v[:, h * 64:(h + 1) * 64], False, hh == 1,
                                     [ams[h], v], [po])
                            ob = osb.next()
                            S.copy("act", ob[:], po[0:64, 0:128], [po], [ob])
                            S.dma("sp", Od[dd][t0 + c * 64:t0 + (c + 1) * 64, p * 128:(p + 1) * 128], ob[:], reads=[ob], writes=[Od[dd]])
                            sn = self.bank()
                            S.mm(sn[:, 0:128], pr[cs, 3, p * 128:(p + 1) * 128], v[cs, p * 128:(p + 1) * 128], True, True, [pr, v], [sn])
                            eg = egt_ap(p, ecol)
                            for hh in range(2):
                                hs = slice(hh * 64, (hh + 1) * 64)
                                S.stt("dve", Sblk[p][hs, hs], Sblk[p][hs, hs], eg[0][hs], sn[hs, hs], ALU.mult, ALU.add,
                                      [Sblk[p], eg[1], sn], [Sblk[p]])
            S.barrier()
        nwsrc = d["hnw_rep"][:, l, :] if which == "hgrn" else None
        self.readout_heads(Od, nwsrc, TM_HG if which == "hgrn" else TM_RG, mrow)

    def readout_heads(self, Od, nwsrc, gcol, mrow):
        S = self.S
        NT = self.NT
        with ExitStack() as es:
            if nwsrc is not None:
                NW = S.sb("nw", [128, 64], es=es)
                S.dma("sp", NW[:], nwsrc, writes=[NW])
            of = Rot([S.sb("of", [128, 256], es=es) for _ in range(2)])
            ob_ = Rot([S.sb("ob", [128, 256], es=es) for _ in range(2)])
            gt = Rot([S.sb("gt", [128, 256], es=es) for _ in range(2)])
            sq = Rot([S.sb("sqr", [128, 256], es=es) for _ in range(2)])
            ss = Rot([S.sb("ss", [128, 4], es=es) for _ in range(2)])
            mt = Rot([S.sb("mt", [128, 2, 128], es=es) for _ in range(2)])
            for n in range(NT):
                t0 = n * 128
                a, b, g, s2, s4, m_ = of.next(), ob_.next(), gt.next(), sq.next(), ss.next(), mt.next()
                S.dma("sp", a[:], Od[0][t0:t0 + 128, :], reads=[Od[0]], writes=[a])
                S.dma("sp", b[:], Od[1][t0:t0 + 128, :], reads=[Od[1]], writes=[b])
                S.dma("sp", g[:], self.PT[t0:t0 + 128, gcol:gcol + 256], reads=[self.PT], writes=[g])
                S.tt("dve", a[:], a[:], b[:], ALU.add, [a, b], [a])
                S.tt("pool", s2[:], a[:], a[:], ALU.mult, [a], [s2])
                S.op("dve", lambda e, s4=s4, s2=s2: e.tensor_reduce(out=s4[:], in_=s2[:].rearrange("p (h e) -> p h e", e=64), axis=AX.X, op=ALU.add), [s2], [s4])
                S.act(s4[:], s4[:], AF.Ln, [s4, self.eps], [s4], bias=self.eps[:, 1:2], scale=1.0 / 64)
                S.act(s4[:], s4[:], AF.Exp, [s4], [s4], scale=-0.5)
                a3 = a[:].rearrange("p (h e) -> p h e", e=64)
                S.tt("dve", a3, a3, s4[:].unsqueeze(2).to_broadcast([128, 4, 64]), ALU.mult, [a, s4], [a])
                if nwsrc is not None:
                    S.tt("pool", a3, a3, NW[:].unsqueeze(1).to_broadcast([128, 4, 64]), ALU.mult, [a, NW], [a])
                S.act(g[:], g[:], AF.Silu, [g], [g])
                S.tt("dve", a[:], a[:], g[:], ALU.mult, [a, g], [a])
                pt = self.bank()
                for p in range(2):
                    S.tr(pt[:, p * 128:(p + 1) * 128], a[:, p * 128:(p + 1) * 128], self.ident[:], [a, self.ident], [pt])
                S.copy("act", m_[:], pt[:, 0:256].rearrange("p (a b) -> p a b", a=2), [pt], [m_])
                for p in range(2):
                    S.dma("sp", self.MIXT[mrow + p * 128:mrow + (p + 1) * 128, t0:t0 + 128], m_[:, p, :], reads=[m_], writes=[self.MIXT])
            S.barrier()

    def sin_of(self, dst, dst_t, x, x_t, tf, ti, shift=0.0):
        S = self.S
        TWO_PI = 2 * math.pi
        if shift != 0.0:
            S.ts("dve", tf[1][:], x, shift, ALU.add, [x_t], [tf[1]])
            x, x_t = tf[1][:], tf[1]
        S.ts("dve", tf[0][:], x, 1.0 / TWO_PI, ALU.mult, [x_t], [tf[0]])
        S.copy("dve", ti[:], tf[0][:], [tf[0]], [ti])
        S.copy("dve", tf[0][:], ti[:], [ti], [tf[0]])
        S.stt("dve", tf[0][:], tf[0][:], -TWO_PI, x, ALU.mult, ALU.add, [tf[0], x_t], [tf[0]])
        S.act(dst, tf[0][:], AF.Sin, [tf[0]], [dst_t])

    def mixer_s5(self, l):
        S = self.S
        d = self.din
        T_ = self.T
        NT = self.NT
        PI = math.pi
        Yd = [S.dram("Y_s5_f", [256, T_]), S.dram("Y_s5_b", [256, T_])]
        with ExitStack() as es:
            LBm = S.sb("LBm", [128, 32, 192], es=es)
            ONES = S.sb("ONES", [128, 128], es=es)
            S.dma("sp", ONES[:], d["c_ones"][:, :], writes=[ONES])
            sgn = S.sb("sgn", [128, 2], es=es)
            S.dma("sp", sgn[:], d["c_sgn"][:, :], writes=[sgn])
            CARRY = S.sb("CARRY", [128, 32], es=es)
            npi = S.sb("npi", [128, 1], es=es)
            S.memset("dve", npi[:], -PI, [npi])
            with ExitStack() as es2:
                lam = S.sb("lam", [32, 2, 64], es=es2)
                stp = S.sb("stp", [32, 1], es=es2)
                S.dma("sp", lam[:], d["s5_lam"][:, l, :, :], writes=[lam])
                S.dma("sp", stp[:], d["s5_lstep"][l], writes=[stp])
                S.act(stp[:], stp[:], AF.Exp, [stp], [stp])
                t = {n: S.sb("s5t_" + n, [32, 64], es=es2) for n in ("r", "th", "x", "sn", "cs", "nr", "ni", "den", "cr", "ci", "tmp")}
                S.ts("dve", t["r"][:], lam[:, 0, :], stp[:, 0:1], ALU.mult, [lam, stp], [t["r"]])
                S.act(t["r"][:], t["r"][:], AF.Exp, [t["r"]], [t["r"]])
                S.ts("dve", t["th"][:], lam[:, 1, :], stp[:, 0:1], ALU.mult, [lam, stp], [t["th"]])
                tf32 = [S.sb("tf32", [32, 64], es=es2) for _ in range(2)]
                ti32 = S.sb("ti32", [32, 64], mybir.dt.int32, es=es2)
                self.sin_of(t["sn"][:], t["sn"], t["th"][:], t["th"], tf32, ti32)
                self.sin_of(t["cs"][:], t["cs"], t["th"][:], t["th"], tf32, ti32, shift=0.5 * PI)
                S.tt("dve", t["nr"][:], t["r"][:], t["cs"][:], ALU.mult, [t["r"], t["cs"]], [t["nr"]])
                S.ts("dve", t["nr"][:], t["nr"][:], -1.0, ALU.add, [t["nr"]], [t["nr"]])
                S.tt("dve", t["ni"][:], t["r"][:], t["sn"][:], ALU.mult, [t["r"], t["sn"]], [t["ni"]])
                S.tt("dve", t["den"][:], lam[:, 0, :], lam[:, 0, :], ALU.mult, [lam], [t["den"]])
                S.tt("dve", t["tmp"][:], lam[:, 1, :], lam[:, 1, :], ALU.mult, [lam], [t["tmp"]])
                S.tt("dve", t["den"][:], t["den"][:], t["tmp"][:], ALU.add, [t["den"], t["tmp"]], [t["den"]])
                S.op("dve", lambda e: e.reciprocal(out=t["den"][:], in_=t["den"][:]), [t["den"]], [t["den"]])
                S.tt("dve", t["cr"][:], t["nr"][:], lam[:, 0, :], ALU.mult, [t["nr"], lam], [t["cr"]])
                S.tt("dve", t["tmp"][:], t["ni"][:], lam[:, 1, :], ALU.mult, [t["ni"], lam], [t["tmp"]])
                S.tt("dve", t["cr"][:], t["cr"][:], t["tmp"][:], ALU.add, [t["cr"], t["tmp"]], [t["cr"]])
                S.tt("dve", t["cr"][:], t["cr"][:], t["den"][:], ALU.mult, [t["cr"], t["den"]], [t["cr"]])
                S.tt("dve", t["ci"][:], t["ni"][:], lam[:, 0, :], ALU.mult, [t["ni"], lam], [t["ci"]])
                S.tt("dve", t["tmp"][:], t["nr"][:], lam[:, 1, :], ALU.mult, [t["nr"], lam], [t["tmp"]])
                S.tt("dve", t["ci"][:], t["ci"][:], t["tmp"][:], ALU.subtract, [t["ci"], t["tmp"]], [t["ci"]])
                S.tt("dve", t["ci"][:], t["ci"][:], t["den"][:], ALU.mult, [t["ci"], t["den"]], [t["ci"]])
                delta = S.sb("delta", [32, 32, 64], es=es2)
                S.dma("sp", delta[:], d["c_delta32"][:, :, :], writes=[delta])
                Dm = S.sb("Dm", [32, 2, 32, 64], es=es2)
                S.tt("dve", Dm[:, 0, :, :], delta[:], t["cr"][:].unsqueeze(1).to_broadcast([32, 32, 64]), ALU.mult, [delta, t["cr"]], [Dm])
                S.tt("dve", Dm[:, 1, :, :], delta[:], t["ci"][:].unsqueeze(1).to_broadcast([32, 32, 64]), ALU.mult, [delta, t["ci"]], [Dm])
                cbc = S.sb("cbc", [128, 2, 32, 64], es=es2)
                for ri in range(2):
                    for cc in range(4):
                        ps = self.bank()
                        S.mm(ps[:, 0:512], ONES[0:32, :], Dm[:, ri, cc * 8:(cc + 1) * 8, :].rearrange("k a b -> k (a b)"), True, True, [ONES, Dm], [ps])
                        S.copy("act", cbc[:, ri, cc * 8:(cc + 1) * 8, :].rearrange("k a b -> k (a b)"), ps[:, 0:512], [ps], [cbc])
                BT = S.sb("BT", [128, 2, 32, 64], es=es2)
                S.dma("sp", BT[:], d["s5_BT"][:, l, :, :, :], writes=[BT])
                t1 = S.sb("bt1", [128, 32, 64], es=es2)
                t2 = S.sb("bt2", [128, 32, 64], es=es2)
                S.tt("dve", t1[:], BT[:, 0, :, :], cbc[:, 0, :, :], ALU.mult, [BT, cbc], [t1])
                S.tt("pool", t2[:], BT[:, 1, :, :], cbc[:, 1, :, :], ALU.mult, [BT, cbc], [t2])
                S.tt("dve", LBm[:, :, 0:64], t1[:], t2[:], ALU.subtract, [t1, t2], [LBm])
                S.copy("act", LBm[:, :, 128:192], LBm[:, :, 0:64], [LBm], [LBm])
                S.tt("dve", t1[:], BT[:, 0, :, :], cbc[:, 1, :, :], ALU.mult, [BT, cbc], [t1])
                S.tt("pool", t2[:], BT[:, 1, :, :], cbc[:, 0, :, :], ALU.mult, [BT, cbc], [t2])
                S.tt("dve", LBm[:, :, 64:128], t1[:], t2[:], ALU.add, [t1, t2], [LBm])
                S.barrier()
            CT1 = S.sb("CT1", [128, 32, 128], es=es)
            CT2 = S.sb("CT2", [128, 32, 128], es=es)
            TinC = S.sb("TinC", [128, 32, 128], es=es)
            TinS = S.sb("TinS", [128, 32, 128], es=es)
            ToutC = S.sb("ToutC", [128, 32, 128], es=es)
            ToutS = S.sb("ToutS", [128, 32, 128], es=es)
            RotM = S.sb("RotM", [128, 32, 128], es=es)
            with ExitStack() as es2:
                S.dma("sp", CT1[:], d["s5_CT"][:, l, 0, :, :], writes=[CT1])
                S.dma("sp", CT2[:], d["s5_CT"][:, l, 1, :, :], writes=[CT2])
                S.ts("dve", CT1[:], CT1[:], sgn[:, 0:1], ALU.mult, [CT1, sgn], [CT1])
                S.ts("dve", CT2[:], CT2[:], sgn[:, 1:2], ALU.mult, [CT2, sgn], [CT2])
                lamT = S.sb("lamT", [128, 2, 32], es=es2)
                S.dma("sp", lamT[:], d["s5_lamT"][:, l, :, :], writes=[lamT])
                stT = S.sb("stT", [128, 32], es=es2)
                S.dma("sp", stT[:], d["s5_lstepT"][:, l, :], writes=[stT])
                S.act(stT[:], stT[:], AF.Exp, [stT], [stT])
                lnr = S.sb("lnr", [128, 32], es=es2)
                nlnr = S.sb("nlnr", [128, 32], es=es2)
                thT = S.sb("thT", [128, 32], es=es2)
                S.tt("dve", lnr[:], lamT[:, 0, :], stT[:], ALU.mult, [lamT, stT], [lnr])
                S.ts("dve", nlnr[:], lnr[:], -1.0, ALU.mult, [lnr], [nlnr])
                S.tt("dve", thT[:], lamT[:, 1, :], stT[:], ALU.mult, [lamT, stT], [thT])
                JP = S.sb("JP", [128, 2, 128], es=es2)
                S.dma("sp", JP[:], d["c_jp1"][:, :, :], writes=[JP])
                xr = Rot([S.sb("xr", [128, 128], es=es2) for _ in range(2)])
                tfT = [S.sb("tfT", [128, 128], es=es2) for _ in range(2)]
                tiT = S.sb("tiT", [128, 128], mybir.dt.int32, es=es2)
                sc_ = Rot([S.sb("sct", [128, 2, 128], es=es2) for _ in range(2)])
                rr = Rot([S.sb("rrt", [128, 2, 128], es=es2) for _ in range(2)])
                for q in range(32):
                    dd = q // 16
                    sct = sc_.next()
                    x = xr.next()
                    S.ts("dve", x[:], JP[:, dd, :], thT[:, q:q + 1], ALU.mult, [JP, thT], [x])
                    self.sin_of(sct[:, 0, :], sct, x[:], x, tfT, tiT)
                    self.sin_of(sct[:, 1, :], sct, x[:], x, tfT, tiT, shift=0.5 * PI)
                    rt = rr.next()
                    S.act(rt[:, 0, :], JP[:, dd, :], AF.Exp, [JP, lnr], [rt], scale=lnr[:, q:q + 1])
                    S.act(rt[:, 1, :], JP[:, dd, :], AF.Exp, [JP, nlnr], [rt], scale=nlnr[:, q:q + 1])
                    S.tt("dve", ToutC[:, q, :], sct[:, 1, :], rt[:, 0, :], ALU.mult, [sct, rt], [ToutC])
                    S.tt("pool", ToutS[:, q, :], sct[:, 0, :], rt[:, 0, :], ALU.mult, [sct, rt], [ToutS])
                    S.tt("dve", TinC[:, q, :], sct[:, 1, :], rt[:, 1, :], ALU.mult, [sct, rt], [TinC])
                    S.stt("dve", TinS[:, q, :], sct[:, 0, :], sgn[:, 0:1], rt[:, 1, :], ALU.mult, ALU.mult, [sct, rt, sgn], [TinS])
                    if dd == 1:
                        S.ts("dve", TinC[:, q, :], TinC[:, q, :], -1.0, ALU.mult, [TinC], [TinC])
                        S.ts("dve", TinS[:, q, :], TinS[:, q, :], -1.0, ALU.mult, [TinS], [TinS])
                PM = S.sb("PM", [128, 2, 128], es=es2)
                S.dma("sp", PM[:], d["c_pmat"][:, :, :], writes=[PM])
                c128 = S.sb("c128", [128, 32], es=es2)
                s128 = S.sb("s128", [128, 32], es=es2)
                ns128 = S.sb("ns128", [128, 32], es=es2)
                S.copy("dve", c128[:, 0:16], ToutC[:, 0:16, 127], [ToutC], [c128])
                S.copy("dve", c128[:, 16:32], ToutC[:, 16:32, 0], [ToutC], [c128])
                S.copy("dve", s128[:, 0:16], ToutS[:, 0:16, 127], [ToutS], [s128])
                S.copy("dve", s128[:, 16:32], ToutS[:, 16:32, 0], [ToutS], [s128])
                S.ts("dve", ns128[:], s128[:], -1.0, ALU.mult, [s128], [ns128])
                for q in range(32):
                    S.ts("dve", RotM[:, q, :], self.ident[:], c128[:, q:q + 1], ALU.mult, [self.ident, c128], [RotM])
                    S.stt("dve", RotM[:, q, :], PM[:, 0, :], ns128[:, q:q + 1], RotM[:, q, :], ALU.mult, ALU.add, [PM, ns128, RotM], [RotM])
                    S.stt("dve", RotM[:, q, :], PM[:, 1, :], s128[:, q:q + 1], RotM[:, q, :], ALU.mult, ALU.add, [PM, s128, RotM], [RotM])
                S.barrier()
            ut = Rot([S.sb("ut", [128, 2, 128], es=es) for _ in range(2)])
            xa = Rot([S.sb("xa", [128, 128], es=es) for _ in range(3)])
            xb_ = Rot([S.sb("xb", [128, 128], es=es) for _ in range(3)])
            vv = Rot([S.sb("vv", [128, 128], es=es) for _ in range(3)])
            Aa = Rot([S.sb("Aa", [128, 8, 128], es=es) for _ in range(2)])
            Bb = Rot([S.sb("Bb", [128, 8, 128], es=es) for _ in range(2)])
            col = Rot([S.sb("col", [128, 2], es=es) for _ in range(4)])
            yst = Rot([S.sb("yst", [128, 128], es=es) for _ in range(2)])
            S.memset("dve", CARRY[:], 0.0, [CARRY])
            for dd in range(2):
                tiles = list(range(NT)) if dd == 0 else [1, 0] + list(range(NT - 1, 1, -1))
                for n in tiles:
                    t0 = n * 128
                    u = ut.next()
                    for kt in range(2):
                        S.dma("sp", u[:, kt, :], self.PF[768 + kt * 128:768 + (kt + 1) * 128, t0:t0 + 128], reads=[self.PF], writes=[u])
                    for kt in range(2):
                        A8, B8 = Aa.next(), Bb.next()
                        for g8 in range(8):
                            q = dd * 16 + kt * 8 + g8
                            pb = self.bank()
                            S.mm(pb[:, 0:128], LBm[:, q, 0:128], u[:, kt, :], True, True, [LBm, u], [pb])
                            S.mm(pb[:, 128:256], LBm[:, q, 64:192], u[:, kt, :], True, True, [LBm, u], [pb])
                            x1, x2, v = xa.next(), xb_.next(), vv.next()
                            S.tt("dve", x1[:], pb[:, 0:128], TinC[:, q, :], ALU.mult, [pb, TinC], [x1])
                            S.tt("dve", x2[:], pb[:, 128:256], TinS[:, q, :], ALU.mult, [pb, TinS], [x2])
                            S.tt("pool", x1[:], x1[:], x2[:], ALU.add, [x1, x2], [x1])
                            if dd == 0:
                                S.op("dve", lambda e, v=v, x1=x1, q=q: e.tensor_tensor_scan(out=v[:], data0=ONES[:], data1=x1[:], initial=CARRY[:, q:q + 1], op0=ALU.mult, op1=ALU.add),
                                     [ONES, x1, CARRY], [v])
                                ccol = v[:, 127:128]
                            else:
                                cl = col.next()
                                S.op("dve", lambda e, cl=cl, x1=x1: e.tensor_reduce(out=cl[:, 0:1], in_=x1[:], axis=AX.X, op=ALU.add), [x1], [cl])
                                S.tt("dve", cl[:, 1:2], CARRY[:, q:q + 1], cl[:, 0:1], ALU.subtract, [CARRY, cl], [cl])
                                S.op("dve", lambda e, v=v, x1=x1, cl=cl: e.tensor_tensor_scan(out=v[:], data0=ONES[:], data1=x1[:], initial=cl[:, 1:2], op0=ALU.mult, op1=ALU.add),
                                     [ONES, x1, cl], [v])
                                S.tt("pool", v[:], v[:], x1[:], ALU.subtract, [v, x1], [v])
                                ccol = v[:, 0:1]
                            pc = self.bank()
                            S.mm(pc[:, 0:1], RotM[:, q, :], ccol, True, True, [RotM, v], [pc])
                            S.copy("act", CARRY[:, q:q + 1], pc[:, 0:1], [pc], [CARRY])
                            S.tt("dve", A8[:, g8, :], v[:], ToutC[:, q, :], ALU.mult, [v, ToutC], [A8])
                            S.tt("pool", B8[:, g8, :], v[:], ToutS[:, q, :], ALU.mult, [v, ToutS], [B8])
                        py = self.bank()
                        for g8 in range(8):
                            q = dd * 16 + kt * 8 + g8
                            S.mm(py[:, 0:128], CT1[:, q, :], A8[:, g8, :], g8 == 0, False, [CT1, A8], [py])
                            S.mm(py[:, 0:128], CT2[:, q, :], B8[:, g8, :], False, g8 == 7, [CT2, B8], [py])
                        ys = yst.next()
                        S.copy("act", ys[:], py[:, 0:128], [py], [ys])
                        S.dma("sp", Yd[dd][kt * 128:(kt + 1) * 128, t0:t0 + 128], ys[:], reads=[ys], writes=[Yd[dd]])
            S.barrier()
        if "S5Y" in self.dbg and l == 0:
            self.dump("S5Yf", Yd[0], [256, T_])
            self.dump("S5Yb", Yd[1], [256, T_])
        with ExitStack() as es:
            gw = S.sb("gw", [128, 2, 256], es=es)
            S.dma("sp", gw[:], d["s5_glu_w"][l].rearrange("(k p) c -> p k c", p=128), writes=[gw])
            dc = S.sb("dc", [128, 2], es=es)
            gb = S.sb("gb", [128, 2], es=es)
            S.dma("sp", dc[:], d["s5_dcol"][:, l, :], writes=[dc])
            S.dma("sp", gb[:], d["s5_gbcol"][:, l, :], writes=[gb])
            yf = Rot([S.sb("yf", [128, 2, 512], es=es) for _ in range(2)])
            yb = Rot([S.sb("yb", [128, 2, 512], es=es) for _ in range(2)])
            uu = Rot([S.sb("uu", [128, 2, 512], es=es) for _ in range(2)])
            zz = Rot([S.sb("zz", [128, 2, 512], es=es) for _ in range(2)])
            tt_ = Rot([S.sb("tt", [128, 512], es=es) for _ in range(3)])
            oo = Rot([S.sb("oo", [128, 512], es=es) for _ in range(3)])
            for (t0, W) in widths(T_):
                a, b, u, z = yf.next(), yb.next(), uu.next(), zz.next()
                for kt in range(2):
                    S.dma("sp", a[:, kt, :W], Yd[0][kt * 128:(kt + 1) * 128, t0:t0 + W], reads=[Yd[0]], writes=[a])
                    S.dma("sp", b[:, kt, :W], Yd[1][kt * 128:(kt + 1) * 128, t0:t0 + W], reads=[Yd[1]], writes=[b])
                    S.dma("sp", u[:, kt, :W], self.PF[768 + kt * 128:768 + (kt + 1) * 128, t0:t0 + W], reads=[self.PF], writes=[u])
                for kt in range(2):
                    S.tt("pool", a[:, kt, :W], a[:, kt, :W], b[:, kt, :W], ALU.add, [a, b], [a])
                    S.stt("dve", a[:, kt, :W], u[:, kt, :W], dc[:, kt:kt + 1], a[:, kt, :W], ALU.mult, ALU.add, [u, dc, a], [a])
                    tq = tt_.next()
                    S.tt("pool", tq[:, :W], a[:, kt, :W], a[:, kt, :W], ALU.mult, [a], [tq])
                    S.ts("dve", tq[:, :W], tq[:, :W], 0.044715, ALU.mult, [tq], [tq], s2=1.0, op1=ALU.add)
                    S.tt("pool", tq[:, :W], tq[:, :W], a[:, kt, :W], ALU.mult, [tq, a], [tq])
                    S.act(tq[:, :W], tq[:, :W], AF.Sigmoid, [tq], [tq], scale=1.5957691216057308)
                    S.tt("dve", z[:, kt, :W], a[:, kt, :W], tq[:, :W], ALU.mult, [a, tq], [z])
                for m in range(2):
                    ps = self.bank()
                    for kt in range(2):
                        S.mm(ps[:, :W], gw[:, kt, m * 128:(m + 1) * 128], z[:, kt, :W], kt == 0, kt == 1, [gw, z], [ps])
                    o = oo.next()
                    S.act(o[:, :W], ps[:, :W], AF.Sigmoid, [ps, gb], [o], bias=gb[:, m:m + 1], scale=1.0)
                    S.tt("dve", o[:, :W], o[:, :W], z[:, m, :W], ALU.mult, [o, z], [o])
                    S.dma("sp", self.MIXT[256 + m * 128:256 + (m + 1) * 128, t0:t0 + W], o[:, :W], reads=[o], writes=[self.MIXT])
            S.barrier()


    def mixer_gdn(self, l):
        S = self.S
        d = self.din
        T_ = self.T
        NT = self.NT
        Od = [S.dram("O_gdn_f", [T_, 256]), S.dram("O_gdn_b", [T_, 256])]
        with ExitStack() as es:
            selh = S.sb("selh", [4, 2, 4, 128], es=es)
            S.dma("sp", selh[:], d["c_selh"][:, :, :, :], writes=[selh])
            negm = S.sb("negm", [128, 2, 3, 128], es=es)
            S.dma("sp", negm[:], d["c_negmask"][:, :, :, :], writes=[negm])
            cw = S.sb("cw", [128, 3, 6], es=es)
            S.dma("sp", cw[:], d["gconv_col"][:, l, :, :], writes=[cw])
            negA = S.sb("negA", [128, 8], es=es)
            S.dma("sp", negA[:], d["galog_rep"][:, l, :], writes=[negA])
            S.act(negA[:], negA[:], AF.Exp, [negA], [negA])
            S.ts("dve", negA[:], negA[:], -1.0, ALU.mult, [negA], [negA])
            dtb = S.sb("dtb", [128, 8], es=es)
            S.dma("sp", dtb[:], d["gdtb_rep"][:, l, :], writes=[dtb])
            one = S.sb("one", [128, 1], es=es)
            S.memset("dve", one[:], 1.0, [one])
            cmask = S.sb("cmask", [128, 2], es=es)
            S.dma("sp", cmask[:], d["c_cmask"][:, :], writes=[cmask])
            kdmr = Rot([S.sb("kdm", [128, 2, 256], es=es) for _ in range(2)])
            xin = Rot([S.sb("xin", [128, 6, 130], es=es) for _ in range(2)])
            cv = Rot([S.sb("cv", [128, 6, 128], es=es) for _ in range(2)])
            tokr = Rot([S.sb("tok", [128, 768], es=es) for _ in range(2)])
            sqr = Rot([S.sb("sqr", [128, 512], es=es) for _ in range(2)])
            rn = Rot([S.sb("rn", [128, 8], es=es) for _ in range(2)])
            qkT = Rot([S.sb("qkT", [128, 4, 128], es=es) for _ in range(2)])
            abr = Rot([S.sb("ab", [128, 16], es=es) for _ in range(2)])
            gsm = Rot([S.sb("gsm", [128, 6, 8], es=es) for _ in range(2)])
            gT = Rot([S.sb("gT", [4, 2, 2, 128], es=es) for _ in range(2)])
            eglr = Rot([S.sb("egl", [64, 2, 4, 2], es=es) for _ in range(2)])
            kvr = Rot([S.sb("kv", [128, 2, 4, 128], es=es) for _ in range(2)])
            kdr = Rot([S.sb("kd", [128, 2, 256], es=es) for _ in range(2)])
            qgr = Rot([S.sb("qg", [128, 2, 256], es=es) for _ in range(2)])
            qgT = Rot([S.sb("qgT", [128, 2, 2, 128], es=es) for _ in range(2)])
            NU = 8
            Fm = [S.sb("Fm%d" % u, [128, 3, 128], es=es) for u in range(NU)]
            Pm = [[S.sb("Pm%d_%d" % (u, i), [128, 128], es=es) for i in range(2)] for u in range(NU)]
            Ptm = [[S.sb("Ptm%d_%d" % (u, i), [128, 128], es=es) for i in range(2)] for u in range(NU)]
            IPm = [S.sb("IPm%d" % u, [128, 128], es=es) for u in range(NU)]
            Ttm = [[S.sb("Ttm%d_%d" % (u, i), [128, 128], es=es) for i in range(2)] for u in range(NU)]
            WU = [S.sb("WU%d" % u, [128, 128], es=es) for u in range(NU)]
            nGT = [S.sb("nGT%d" % u, [64, 2, 64], es=es) for u in range(NU)]
            QeT = [S.sb("QeT%d" % u, [64, 2, 64], es=es) for u in range(NU)]
            Sst = [S.sb("Sst%d" % u, [64, 64], es=es) for u in range(NU)]
            osb = Rot([S.sb("osb", [64, 64], es=es) for _ in range(6)])
            evq = Rot(["dve", "act", "pool"])
            evq2 = Rot(["dve", "act"])

            def evac(out, in_, reads, writes, psum=True):
                e = evq2.next() if psum else evq.next()
                S.copy(e, out, in_, reads, writes)

            for u in range(NU):
                S.memset("dve", Sst[u][:], 0.0, [Sst[u]])
            for dd in range(2):
                tiles = list(range(NT)) if dd == 0 else [1, 0] + list(range(NT - 1, 1, -1))
                chunks = [0, 1] if dd == 0 else [1, 0]
                for n in tiles:
                    t0 = n * 128
                    x = xin.next()
                    left_zero = (t0 == 0) or (t0 == CTX)
                    right_zero = (t0 + 128 == CTX) or (t0 + 128 == T_)
                    pfv = self.PF.t[0:768, :].rearrange("(c p) t -> p c t", p=128)
                    a0 = t0 - (0 if left_zero else 1)
                    a1 = t0 + 128 + (0 if right_zero else 1)
                    if left_zero:
                        S.memset("pool", x[:, :, 0:1], 0.0, [x])
                    if right_zero:
                        S.memset("pool", x[:, :, 129:130], 0.0, [x])
                    for c in range(6):
                        S.dma("sp", x[:, c, (1 if left_zero else 0):(129 if right_zero else 130)], pfv[:, c, a0:a1], reads=[self.PF], writes=[x])
                    cvt = cv.next()
                    for c in range(6):
                        e = "dve"
                        S.ts(e, cvt[:, c, :], x[:, c, 0:128], cw[:, 0, c:c + 1], ALU.mult, [x, cw], [cvt])
                        S.stt("dve", cvt[:, c, :], x[:, c, 1:129], cw[:, 1, c:c + 1], cvt[:, c, :], ALU.mult, ALU.add, [x, cw, cvt], [cvt])
                        S.stt("dve", cvt[:, c, :], x[:, c, 2:130], cw[:, 2, c:c + 1], cvt[:, c, :], ALU.mult, ALU.add, [x, cw, cvt], [cvt])
                        S.act(cvt[:, c, :], cvt[:, c, :], AF.Silu, [cvt], [cvt])
                    tok = tokr.next()
                    for half in range(2):
                        pt = self.bank()
                        for cc in range(3):
                            c = half * 3 + cc
                            S.tr(pt[:, cc * 128:(cc + 1) * 128], cvt[:, c, :], self.ident[:], [cvt, self.ident], [pt])
                        evac(tok[:, half * 384:(half + 1) * 384], pt[:, 0:384], [pt], [tok])
                    sq_, rn_ = sqr.next(), rn.next()
                    S.tt("pool", sq_[:], tok[:, 0:512], tok[:, 0:512], ALU.mult, [tok], [sq_])
                    S.op("dve", lambda e, rn_=rn_, sq_=sq_: e.tensor_reduce(out=rn_[:], in_=sq_[:].rearrange("p (h e) -> p h e", e=64), axis=AX.X, op=ALU.add), [sq_], [rn_])
                    S.act(rn_[:], rn_[:], AF.Ln, [rn_, self.eps], [rn_], bias=self.eps[:, 1:2], scale=1.0)
                    S.act(rn_[:], rn_[:], AF.Exp, [rn_], [rn_], scale=-0.5)
                    S.ts("dve", rn_[:, 0:4], rn_[:, 0:4], 0.125, ALU.mult, [rn_], [rn_])
                    t3 = tok[:, 0:512].rearrange("p (h e) -> p h e", e=64)
                    S.tt("dve", t3, t3, rn_[:].unsqueeze(2).to_broadcast([128, 8, 64]), ALU.mult, [tok, rn_], [tok])
                    qk = qkT.next()
                    pt = self.bank()
                    for c in range(4):
                        S.tr(pt[:, c * 128:(c + 1) * 128], tok[:, c * 128:(c + 1) * 128], self.ident[:], [tok, self.ident], [pt])
                    evac(qk[:].rearrange("p a b -> p (a b)"), pt[:, 0:512], [pt], [qk])
                    if GDN_STAGE < 2:
                        continue
                    ab = abr.next()
                    S.dma("sp", ab[:], self.PT[t0:t0 + 128, TM_AB:TM_AB + 16], reads=[self.PT], writes=[ab])
                    gs = gsm.next()
                    G_, BE, LNB, EG, EKD, BEG = (gs[:, i, :] for i in range(6))
                    S.tt("dve", G_, ab[:, 0:8], dtb[:], ALU.add, [ab, dtb], [gs])
                    S.act(G_, G_, AF.Exp, [gs], [gs])
                    S.act(G_, G_, AF.Ln, [gs, one], [gs], bias=one[:, 0:1], scale=1.0)
                    S.tt("dve", G_, G_, negA[:], ALU.mult, [gs, negA], [gs])
                    S.act(BE, ab[:, 8:16], AF.Sigmoid, [ab], [gs])
                    S.act(LNB, BE, AF.Ln, [gs], [gs])
                    pg = self.bank()
                    ds_ = slice(dd * 4, dd * 4 + 4)
                    S.mm(pg[:, 0:4], self.cummat[:, dd, 1, :], gs[:, 0, ds_], True, True, [self.cummat, gs], [pg])
                    S.mm(pg[:, 8:12], self.cummat[:, dd, 2, :], gs[:, 0, ds_], True, True, [self.cummat, gs], [pg])
                    S.act(gs[:, 3, ds_], pg[:, 0:4], AF.Exp, [pg], [gs])
                    S.act(gs[:, 4, ds_], pg[:, 8:12], AF.Exp, [pg], [gs])
                    S.tt("dve", gs[:, 5, ds_], gs[:, 3, ds_], gs[:, 1, ds_], ALU.mult, [gs], [gs])
                    gt_ = gT.next()
                    pgt = self.bank()
                    S.mm(pgt[0:4, 0:128], gs[:, 0, ds_], self.cummat[:, dd, 1, :], True, True, [gs, self.cummat], [pgt])
                    evac(gt_[:, dd, 0, :], pgt[0:4, 0:128], [pgt], [gt_])
                    pgt2 = self.bank()
                    S.mm(pgt2[0:4, 0:128], gs[:, 0, ds_], self.cummat[:, dd, 1, :], True, False, [gs, self.cummat], [pgt2])
                    S.mm(pgt2[0:4, 0:128], gs[:, 2, ds_], self.ident[:], False, True, [gs, self.ident], [pgt2])
                    evac(gt_[:, dd, 1, :], pgt2[0:4, 0:128], [pgt2], [gt_])
                    egl = eglr.next()
                    lastcol = 63 if dd == 0 else 0
                    for h in range(4):
                        pe_ = self.bank()
                        S.mm(pe_[0:64, 0:2], selh[:, 0, h, 0:64], gt_[:, dd, 0, lastcol:lastcol + 65:64], True, True, [selh, gt_], [pe_])
                        S.act(egl[:, dd, h, :], pe_[0:64, 0:2], AF.Exp, [pe_], [egl])
                    if GDN_STAGE < 3:
                        continue
                    kv, kd, qg = kvr.next(), kdr.next(), qgr.next()
                    k3 = tok[:, 256:512].rearrange("p (h e) -> p h e", e=64)
                    q3 = tok[:, 0:256].rearrange("p (h e) -> p h e", e=64)
                    v3 = tok[:, 512:768].rearrange("p (h e) -> p h e", e=64)
                    bc = lambda i: gs[:, i, ds_].unsqueeze(2).to_broadcast([128, 4, 64])
                    S.tt("dve", kv[:, dd, :, 0:64], k3, bc(5), ALU.mult, [tok, gs], [kv])
                    S.tt("pool", kv[:, dd, :, 64:128], v3, bc(1), ALU.mult, [tok, gs], [kv])
                    S.tt("dve", kd[:, dd, :].rearrange("p (h e) -> p h e", e=64), k3, bc(4), ALU.mult, [tok, gs], [kd])
                    S.tt("pool", qg[:, dd, :].rearrange("p (h e) -> p h e", e=64), q3, bc(3), ALU.mult, [tok, gs], [qg])
                    qgt = qgT.next()
                    pt = self.bank()
                    for p in range(2):
                        S.tr(pt[:, p * 128:(p + 1) * 128], qg[:, dd, p * 128:(p + 1) * 128], self.ident[:], [qg, self.ident], [pt])
                    evac(qgt[:, dd, :, :].rearrange("p a b -> p (a b)"), pt[:, 0:256], [pt], [qgt])
                    if GDN_STAGE < 4:
                        continue
                    for h in range(4):
                        u = dd * 4 + h
                        p, hh = h // 2, h % 2
                        hs_ = slice(64 * hh, 64 * hh + 64)
                        kT_h = qk[hs_, 2 + p, :]
                        qT_h = qk[hs_, p, :]
                        F = Fm[u]
                        pf = self.bank()
                        S.mm(pf[:, 0:128], gt_[:, dd, 1, :], selh[:, 0, h, :], True, False, [gt_, selh], [pf])
                        S.mm(pf[:, 0:128], selh[:, 1, h, :], gt_[:, dd, 0, :], False, False, [gt_, selh], [pf])
                        S.mm(pf[:, 0:128], self.ident[:], negm[:, dd, 0, :], False, True, [self.ident, negm], [pf])
                        S.act(F[:, 0, :], pf[:, 0:128], AF.Exp, [pf], [F])
                        pf = self.bank()
                        S.mm(pf[:, 0:128], selh[:, 0, h, :], gt_[:, dd, 1, :], True, False, [gt_, selh], [pf])
                        S.mm(pf[:, 0:128], gt_[:, dd, 0, :], selh[:, 1, h, :], False, False, [gt_, selh], [pf])
                        S.mm(pf[:, 0:128], self.ident[:], negm[:, dd, 1, :], False, True, [self.ident, negm], [pf])
                        S.act(F[:, 1, :], pf[:, 0:128], AF.Exp, [pf], [F])
                        pf = self.bank()
                        S.mm(pf[:, 0:128], selh[:, 0, h, :], gt_[:, dd, 0, :], True, False, [gt_, selh], [pf])
                        S.mm(pf[:, 0:128], gt_[:, dd, 0, :], selh[:, 1, h, :], False, False, [gt_, selh], [pf])
                        S.mm(pf[:, 0:128], self.ident[:], negm[:, dd, 2, :], False, True, [self.ident, negm], [pf])
                        S.act(F[:, 2, :], pf[:, 0:128], AF.Exp, [pf], [F])
                        pk = self.bank()
                        S.mm(pk[:, 0:128], kT_h, kT_h, True, True, [qk], [pk])
                        S.stt("dve", Pm[u][0][:], pk[:, 0:128], -1.0, F[:, 0, :], ALU.mult, ALU.mult, [pk, F], [Pm[u][0]])
                        S.stt("dve", Ptm[u][0][:], pk[:, 0:128], -1.0, F[:, 1, :], ALU.mult, ALU.mult, [pk, F], [Ptm[u][0]])
                        S.tt("pool", Ttm[u][0][:], Ptm[u][0][:], self.ident[:], ALU.add, [Ptm[u][0], self.ident], [Ttm[u][0]])
                        pq = self.bank()
                        S.mm(pq[:, 0:128], kT_h, qT_h, True, True, [qk], [pq])
                        S.tt("dve", F[:, 2, :], pq[:, 0:128], F[:, 2, :], ALU.mult, [pq, F], [F])
                    if GDN_STAGE < 5:
                        continue
                    for k in range(1, 6):
                        a_, b_ = (k - 1) % 2, k % 2
                        for h in range(4):
                            u = dd * 4 + h
                            pp = self.bank()
                            S.mm(pp[:, 0:128], Ptm[u][a_][:], Pm[u][a_][:], True, True, [Ptm[u][a_], Pm[u][a_]], [pp])
                            if k < 5:
                                evac(Pm[u][b_][:], pp[:, 0:128], [pp], [Pm[u][b_]])
                            S.tt("dve", IPm[u][:], pp[:, 0:128], self.ident[:], ALU.add, [pp, self.ident], [IPm[u]])
                            if k < 5:
                                pp2 = self.bank()
                                S.mm(pp2[:, 0:128], Pm[u][a_][:], Ptm[u][a_][:], True, True, [Ptm[u][a_], Pm[u][a_]], [pp2])
                                evac(Ptm[u][b_][:], pp2[:, 0:128], [pp2], [Ptm[u][b_]])
                            pt_ = self.bank()
                            S.mm(pt_[:, 0:128], IPm[u][:], Ttm[u][a_][:], True, True, [IPm[u], Ttm[u][a_]], [pt_])
                            evac(Ttm[u][b_][:], pt_[:, 0:128], [pt_], [Ttm[u][b_]])
                    if GDN_STAGE < 6:
                        continue
                    for h in range(4):
                        u = dd * 4 + h
                        pw = self.bank()
                        S.mm(pw[:, 0:128], Ttm[u][1][:], kv[:, dd, h, :], True, True, [Ttm[u][1], kv], [pw])
                        evac(WU[u][:], pw[:, 0:128], [pw], [WU[u]])
                    for h in range(4):
                        u = dd * 4 + h
                        p, hh = h // 2, h % 2
                        for c in range(2):
                            cs = slice(64 * c, 64 * c + 64)
                            pg_ = self.bank()
                            S.mm(pg_[0:64, 0:64], WU[u][cs, 0:64], kd[cs, dd, h * 64:(h + 1) * 64], True, True, [WU[u], kd], [pg_])
                            S.ts("dve", nGT[u][:, c, :], pg_[0:64, 0:64], -1.0, ALU.mult, [pg_], [nGT[u]])
                            pq_ = self.bank()
                            S.mm(pq_[0:64, 0:64], WU[u][cs, 0:64], Fm[u][cs, 2, cs], True, True, [WU[u], Fm[u]], [pq_])
                            if hh == 0:
                                S.tt("dve", QeT[u][:, c, :], qgt[0:64, dd, p, cs], pq_[0:64, 0:64], ALU.subtract, [qgt, pq_], [QeT[u]])
                            else:
                                tmpq = osb.next()
                                S.copy("dve", tmpq[:], qgt[64:128, dd, p, cs], [qgt], [tmpq])
                                S.tt("dve", QeT[u][:, c, :], tmpq[:], pq_[0:64, 0:64], ALU.subtract, [tmpq, pq_], [QeT[u]])
                    if GDN_STAGE < 7:
                        continue
                    kdm = kdmr.next()
                    for c in range(2):
                        S.ts("dve" if c == 0 else "pool", kdm[:, c, :], kd[:, dd, :], cmask[:, c:c + 1], ALU.mult, [kd, cmask], [kdm])
                    for c in chunks:
                        cs = slice(64 * c, 64 * c + 64)
                        for h in range(4):
                            u = dd * 4 + h
                            po = self.bank()
                            S.mm(po[0:64, 0:64], QeT[u][:, c, :], Sst[u][:], True, False, [QeT[u], Sst[u]], [po])
                            S.mm(po[0:64, 0:64], Fm[u][:, 2, cs], WU[u][:, 64:128], False, True, [Fm[u], WU[u]], [po])
                            ob = osb.next()
                            S.copy("act", ob[:], po[0:64, 0:64], [po], [ob])
                            S.dma("sp", Od[dd][t0 + c * 64:t0 + (c + 1) * 64, h * 64:(h + 1) * 64], ob[:], reads=[ob], writes=[Od[dd]])
                            pn = self.bank()
                            S.mm(pn[0:64, 0:64], nGT[u][:, c, :], Sst[u][:], True, False, [nGT[u], Sst[u]], [pn])
                            S.mm(pn[0:64, 0:64], kdm[:, c, h * 64:(h + 1) * 64], WU[u][:, 64:128], False, True, [kdm, WU[u]], [pn])
                            S.stt("dve", Sst[u][:], Sst[u][:], egl[:, dd, h, c:c + 1], pn[0:64, 0:64], ALU.mult, ALU.add,
                                  [Sst[u], egl, pn], [Sst[u]])
            S.barrier()
        self.readout_heads(Od, d["gnw_rep"][:, l, :], TM_GZ, 0)


_CACHE = {}


def run(inputs, nlat, depth, mixers=("gdn", "s5", "hgrn", "ret"), dbg=(), ncores=4, nb=4):
    base = [prep_inputs(inputs, b, nlat, depth) for b in range(nb)]
    in_maps = [base[c % nb] for c in range(ncores)]
    shapes = {k: v.shape for k, v in in_maps[0].items()}
    B = Builder(nlat, depth, shapes, mixers=mixers, dbg=dbg)
    nc = B.build()
    res = run_bass_kernel_spmd(nc, in_maps, core_ids=list(range(ncores)))
    return res, B


def kernel(**inputs):
    nlat = inputs["x"].shape[1]
    depth = inputs["ada_w"].shape[0]
    nb = inputs["x"].shape[0]
    res, B = run(inputs, nlat, depth, ncores=nb, nb=nb)
    out = np.stack([np.ascontiguousarray(res.results[b]["outT"].T) for b in range(nb)], 0)
    return out.astype(np.float32)
```
